# Optimizing a Trainium2 kernel written in Bass

```python
import math
import jax
import jax.numpy as jnp
from jax import lax

D_MODEL = 1024
BATCH = 2
SEQ = 8192
DEPTH = 2

GRID_W = 64
CTX_LEN = 256
N_MOD = 6

S5_WIDTH = 768
S5_GROUP = 16
S5_GROUPS = S5_WIDTH // S5_GROUP
S5_STATE = 64

LRU_WIDTH = 1024
LRU_BLOCKS = 16
LRU_BLOCK = LRU_WIDTH // LRU_BLOCKS
LRU_CONV = 4
LRU_C = 8.0

SSD_INNER = 1024
SSD_HEAD_DIM = 64
SSD_HEADS = SSD_INNER // SSD_HEAD_DIM
SSD_GROUPS = 4
SSD_HPG = SSD_HEADS // SSD_GROUPS
SSD_STATE = 128
SSD_CONV = 4
SSD_CHUNK = 128
SSD_XBC = SSD_INNER + 2 * SSD_GROUPS * SSD_STATE

N_BRANCH = 3
IN_SIZES = (S5_WIDTH, LRU_WIDTH, LRU_WIDTH, SSD_INNER, SSD_XBC, SSD_HEADS, N_BRANCH * D_MODEL)
IN_TOTAL = sum(IN_SIZES)
IN_SPLITS = tuple(sum(IN_SIZES[: i + 1]) for i in range(len(IN_SIZES) - 1))

D_FF = 2816
N_EXPERTS = 8
TOP_K = 2
D_FF_EXPERT = 3584
N_DENSE = (DEPTH + 1) // 2
N_MOE = DEPTH // 2
EPS = 1e-6

kernel_name = 'hybrid_s5_rglru_ssd_moe_dit'


def rmsnorm(x, w):
    xf = x.astype(jnp.float32)
    y = xf * lax.rsqrt(jnp.mean(xf * xf, axis=-1, keepdims=True) + EPS)
    return (y * w.astype(jnp.float32)).astype(x.dtype)


def modulate(h, shift, scale):
    return h * (1.0 + scale) + shift


def dwconv(x, w, b):
    k, ch = w.shape
    lo = k // 2
    y = lax.conv_general_dilated(x, w.astype(x.dtype)[:, None, :], window_strides=(1,),
                                 padding=((lo, k - 1 - lo),), dimension_numbers=('NWC', 'WIO', 'NWC'),
                                 feature_group_count=ch)
    return y + b.astype(x.dtype)


def to_col_major(x):
    b, l, ch = x.shape
    rows = l // GRID_W
    return x.reshape(b, rows, GRID_W, ch).transpose(0, 2, 1, 3).reshape(b, l, ch)


def to_row_major(x):
    b, l, ch = x.shape
    rows = l // GRID_W
    return x.reshape(b, GRID_W, rows, ch).transpose(0, 2, 1, 3).reshape(b, l, ch)


def linear_scan(a, b):
    def combine(left, right):
        a_l, b_l = left
        a_r, b_r = right
        return a_r * a_l, a_r * b_l + b_r
    return lax.associative_scan(combine, (a, b), axis=1)


def s5_direction(u_ctx, u_lat, lam_re, lam_im, log_dt, b_re, b_im, c_re, c_im, reverse, ctx_out):
    f32 = jnp.float32
    if reverse:
        u_ctx, u_lat = u_ctx[:, ::-1], u_lat[:, ::-1]
    lam = lax.complex(lam_re.astype(f32), lam_im.astype(f32))
    lam_dt = lam * jnp.exp(log_dt.astype(f32))[:, None]
    a_bar = jnp.exp(lam_dt)
    b_bar = ((a_bar - 1.0) / lam)[:, :, None] * lax.complex(b_re.astype(f32), b_im.astype(f32))
    c_mat = lax.complex(c_re.astype(f32), c_im.astype(f32))

    def run(u, h0):
        bu = jnp.einsum('blgh,gph->blgp', u.astype(jnp.complex64), b_bar)
        _, h = linear_scan(jnp.broadcast_to(a_bar, bu.shape), bu)
        if h0 is not None:
            steps = jnp.arange(1, u.shape[1] + 1, dtype=f32)[:, None, None]
            h = h + jnp.exp(steps * lam_dt)[None] * h0[:, None]
        return h

    def readout(h):
        return jnp.einsum('blgp,ghp->blgh', h, c_mat).real

    h_ctx = run(u_ctx, None)
    h_lat = run(u_lat, h_ctx[:, -1])
    y_lat = readout(h_lat)
    y_ctx = readout(h_ctx) if ctx_out else None
    if reverse:
        y_lat = y_lat[:, ::-1]
        y_ctx = y_ctx[:, ::-1] if ctx_out else None
    return y_lat, y_ctx


def s5_branch(u_ctx, u_lat, lam_re, lam_im, log_dt, b_re, b_im, c_re, c_im, d_skip, w_glu, ctx_out):
    def groups(u):
        return u.astype(jnp.float32).reshape(u.shape[0], u.shape[1], S5_GROUPS, S5_GROUP)
    g_ctx, g_lat = groups(u_ctx), groups(u_lat)
    dirs = [s5_direction(g_ctx, g_lat, lam_re[d], lam_im[d], log_dt[d], b_re[d], b_im[d], c_re[d], c_im[d],
                         d == 1, ctx_out) for d in range(2)]

    def finish(y_fwd, y_bwd, u):
        y = (y_fwd + y_bwd).reshape(u.shape) + d_skip * u.astype(jnp.float32)
        y = jax.nn.gelu(y).astype(u.dtype)
        return y * jax.nn.sigmoid(y @ w_glu)

    y_lat = finish(dirs[0][0], dirs[1][0], u_lat)
    y_ctx = finish(dirs[0][1], dirs[1][1], u_ctx) if ctx_out else None
    return y_lat, y_ctx


def blockdiag(x, w, b):
    xb = x.reshape(x.shape[0], x.shape[1], LRU_BLOCKS, LRU_BLOCK)
    return jnp.einsum('blnk,nkj->blnj', xb, w).reshape(x.shape) + b


def rglru_direction(x_ctx, x_lat, w_a, b_a, w_x, b_x, lam, reverse, ctx_out):
    if reverse:
        x_ctx, x_lat = x_ctx[:, ::-1], x_lat[:, ::-1]
    log_sig = jax.nn.log_sigmoid(lam.astype(jnp.float32))

    def coeffs(x):
        r = jax.nn.sigmoid(blockdiag(x, w_a, b_a))
        i = jax.nn.sigmoid(blockdiag(x, w_x, b_x))
        log_a = LRU_C * r * log_sig
        return jnp.exp(log_a), jnp.sqrt(-jnp.expm1(2.0 * log_a)) * (i * x)

    _, h_ctx = linear_scan(*coeffs(x_ctx))
    a_cum, h_lat = linear_scan(*coeffs(x_lat))
    h_lat = h_lat + a_cum * h_ctx[:, -1:]
    if reverse:
        h_lat = h_lat[:, ::-1]
        h_ctx = h_ctx[:, ::-1]
    return h_lat, (h_ctx if ctx_out else None)


def rglru_branch(x_ctx, x_lat, g_ctx, g_lat, conv_w, conv_b, w_a, b_a, w_x, b_x, lam, ctx_out):
    f32 = jnp.float32
    xc = dwconv(x_ctx.astype(f32), conv_w, conv_b)
    xl = dwconv(to_col_major(x_lat.astype(f32)), conv_w, conv_b)
    dirs = [rglru_direction(xc, xl, w_a[d], b_a[d], w_x[d], b_x[d], lam[d], d == 1, ctx_out) for d in range(2)]
    y_lat = to_row_major(dirs[0][0] + dirs[1][0]).astype(x_lat.dtype) * jax.nn.gelu(g_lat)
    y_ctx = ((dirs[0][1] + dirs[1][1]).astype(x_ctx.dtype) * jax.nn.gelu(g_ctx)) if ctx_out else None
    return y_lat, y_ctx


def segsum_exp(a):
    cs = jnp.cumsum(a, axis=-1)
    n = a.shape[-1]
    mask = jnp.tril(jnp.ones((n, n), dtype=bool))
    return jnp.exp(jnp.where(mask, cs[..., :, None] - cs[..., None, :], -jnp.inf))


def ssd_scan(x, dt, a, bm, cm, h0, with_y):
    bsz, n_tok = x.shape[0], x.shape[1]
    nc = n_tok // SSD_CHUNK
    shp = (bsz, nc, SSD_CHUNK, SSD_GROUPS, SSD_HPG)
    xc = (x * dt[..., None]).reshape(shp + (SSD_HEAD_DIM,))
    a_dt = (dt * a).reshape(shp).transpose(0, 3, 4, 1, 2)
    bc = bm.reshape(bsz, nc, SSD_CHUNK, SSD_GROUPS, SSD_STATE)
    cc = cm.reshape(bsz, nc, SSD_CHUNK, SSD_GROUPS, SSD_STATE)
    a_cs = jnp.cumsum(a_dt, axis=-1)
    decay_to_end = jnp.exp(a_cs[..., -1:] - a_cs).transpose(0, 3, 4, 1, 2)
    states = jnp.einsum('bcsgn,bcsgjp->bcgjpn', bc, xc * decay_to_end[..., None])
    chunk_decay = jnp.exp(a_cs[..., -1])

    def step(h, inp):
        st, dec = inp
        return dec[..., None, None] * h + st, h

    h_last, h_prev = lax.scan(step, h0, (jnp.moveaxis(states, 1, 0), jnp.moveaxis(chunk_decay, 3, 0)))
    if not with_y:
        return None, h_last
    h_prev = jnp.moveaxis(h_prev, 0, 1)
    scores = jnp.einsum('bclgn,bcsgn->bgcls', cc, bc)
    y_diag = jnp.einsum('bgjcls,bcsgjp->bclgjp', scores[:, :, None] * segsum_exp(a_dt), xc)
    y_off = jnp.einsum('bclgn,bcgjpn,bgjcl->bclgjp', cc, h_prev, jnp.exp(a_cs))
    return (y_diag + y_off).reshape(bsz, n_tok, SSD_HEADS, SSD_HEAD_DIM), h_last


def ssd_direction(ctx_in, lat_in, a_log, dt_bias, reverse, ctx_out):
    if reverse:
        ctx_in = tuple(t[:, ::-1] for t in ctx_in)
        lat_in = tuple(t[:, ::-1] for t in lat_in)
    a = -jnp.exp(a_log.astype(jnp.float32))

    def run(inp, h0, with_y):
        xs, bm, cm, dt_raw = inp
        dt = jax.nn.softplus(dt_raw + dt_bias.astype(jnp.float32))
        return ssd_scan(xs, dt, a, bm, cm, h0, with_y)

    bsz = lat_in[0].shape[0]
    h0 = jnp.zeros((bsz, SSD_GROUPS, SSD_HPG, SSD_HEAD_DIM, SSD_STATE), jnp.float32)
    y_ctx, h_ctx = run(ctx_in, h0, ctx_out)
    y_lat, _ = run(lat_in, h_ctx, True)
    if reverse:
        y_lat = y_lat[:, ::-1]
        y_ctx = y_ctx[:, ::-1] if ctx_out else None
    return y_lat, y_ctx


def ssd_branch(z_ctx, xbc_ctx, dt_ctx, z_lat, xbc_lat, dt_lat, conv_w, conv_b, a_log, dt_bias, d_skip, norm_w,
               ctx_out):
    f32 = jnp.float32

    def prep(xbc, dt_raw):
        xbc = jax.nn.silu(dwconv(xbc.astype(f32), conv_w, conv_b))
        xs, bm, cm = jnp.split(xbc, (SSD_INNER, SSD_INNER + SSD_GROUPS * SSD_STATE), axis=-1)
        b, l = xs.shape[0], xs.shape[1]
        return (xs.reshape(b, l, SSD_HEADS, SSD_HEAD_DIM), bm.reshape(b, l, SSD_GROUPS, SSD_STATE),
                cm.reshape(b, l, SSD_GROUPS, SSD_STATE), dt_raw.astype(f32))

    ctx_in, lat_in = prep(xbc_ctx, dt_ctx), prep(xbc_lat, dt_lat)
    dirs = [ssd_direction(ctx_in, lat_in, a_log[d], dt_bias[d], d == 1, ctx_out) for d in range(2)]

    def finish(y_fwd, y_bwd, inp, z):
        xs = inp[0]
        y = y_fwd + y_bwd + d_skip.astype(f32)[:, None] * xs
        y = y.reshape(xs.shape[0], xs.shape[1], SSD_INNER).astype(z.dtype)
        return rmsnorm(y * jax.nn.silu(z), norm_w)

    y_lat = finish(dirs[0][0], dirs[1][0], lat_in, z_lat)
    y_ctx = finish(dirs[0][1], dirs[1][1], ctx_in, z_ctx) if ctx_out else None
    return y_lat, y_ctx


def token_mixer(h_ctx, h_lat, w_in, s5_lam_re, s5_lam_im, s5_log_dt, s5_b_re, s5_b_im, s5_c_re, s5_c_im, s5_d,
                s5_w_glu, lru_conv_w, lru_conv_b, lru_w_a, lru_b_a, lru_w_x, lru_b_x, lru_lam, ssd_conv_w,
                ssd_conv_b, ssd_a_log, ssd_dt_bias, ssd_d, ssd_norm_w, w_br_a, w_br_b, w_br_c, w_out, ctx_out):
    pc = jnp.split(h_ctx @ w_in, IN_SPLITS, axis=-1)
    pl = jnp.split(h_lat @ w_in, IN_SPLITS, axis=-1)
    ya_lat, ya_ctx = s5_branch(pc[0], pl[0], s5_lam_re, s5_lam_im, s5_log_dt, s5_b_re, s5_b_im, s5_c_re,
                               s5_c_im, s5_d, s5_w_glu, ctx_out)
    yb_lat, yb_ctx = rglru_branch(pc[1], pl[1], pc[2], pl[2], lru_conv_w, lru_conv_b, lru_w_a, lru_b_a,
                                  lru_w_x, lru_b_x, lru_lam, ctx_out)
    yc_lat, yc_ctx = ssd_branch(pc[3], pc[4], pc[5], pl[3], pl[4], pl[5], ssd_conv_w, ssd_conv_b, ssd_a_log,
                                ssd_dt_bias, ssd_d, ssd_norm_w, ctx_out)

    def merge(ya, yb, yc, gates):
        ga, gb, gc = jnp.split(jax.nn.sigmoid(gates), N_BRANCH, axis=-1)
        return (ga * (ya @ w_br_a) + gb * (yb @ w_br_b) + gc * (yc @ w_br_c)) @ w_out

    y_lat = merge(ya_lat, yb_lat, yc_lat, pl[6])
    y_ctx = merge(ya_ctx, yb_ctx, yc_ctx, pc[6]) if ctx_out else None
    return y_lat, y_ctx


def swiglu(h, w1, w3, w2):
    return (jax.nn.silu(h @ w1) * (h @ w3)) @ w2


def moe_swiglu(h, w_router, b_router, w1, w3, w2):
    logits = (h @ w_router).astype(jnp.float32) + b_router.astype(jnp.float32)
    top_logit, top_idx = lax.top_k(logits, TOP_K)
    weights = jax.nn.softmax(top_logit, axis=-1)
    gate = jnp.sum(jax.nn.one_hot(top_idx, N_EXPERTS, dtype=jnp.float32) * weights[..., None], axis=-2)
    gate = gate.astype(h.dtype)
    out = jnp.zeros_like(h)
    for e in range(N_EXPERTS):
        out = out + gate[..., e:e + 1] * swiglu(h, w1[e], w3[e], w2[e])
    return out


def setup_inputs(seed: int = 0) -> dict:
    key = jax.random.key(seed)
    ks = iter(jax.random.split(key, 64))
    f32 = jnp.float32

    def nrm(shape, scale):
        return jax.random.normal(next(ks), shape, f32) * scale

    def unif(shape, lo, hi):
        return jax.random.uniform(next(ks), shape, f32, lo, hi)

    x = nrm((BATCH, SEQ, D_MODEL), 1.0)
    c = nrm((BATCH, D_MODEL), 1.0)
    ctx = nrm((BATCH, CTX_LEN, D_MODEL), 1.0)
    c_ctx = nrm((D_MODEL,), 1.0)
    w_mod = nrm((DEPTH, D_MODEL, N_MOD * D_MODEL), 0.5 * D_MODEL ** -0.5)
    b_mod = nrm((DEPTH, N_MOD * D_MODEL), 0.02)
    norm_w = 1.0 + nrm((DEPTH, 2, D_MODEL), 0.05)
    w_in = nrm((DEPTH, D_MODEL, IN_TOTAL), D_MODEL ** -0.5)
    s5_shape = (DEPTH, 2, S5_GROUPS, S5_STATE)
    s5_lam_re = -0.5 + nrm(s5_shape, 0.01)
    s5_lam_im = math.pi * jnp.arange(S5_STATE, dtype=f32) + nrm(s5_shape, 0.01)
    s5_log_dt = unif((DEPTH, 2, S5_GROUPS), math.log(1e-3), math.log(1e-1))
    s5_b_re = nrm((DEPTH, 2, S5_GROUPS, S5_STATE, S5_GROUP), (2 * S5_GROUP) ** -0.5)
    s5_b_im = nrm((DEPTH, 2, S5_GROUPS, S5_STATE, S5_GROUP), (2 * S5_GROUP) ** -0.5)
    s5_c_re = nrm((DEPTH, 2, S5_GROUPS, S5_GROUP, S5_STATE), 0.25)
    s5_c_im = nrm((DEPTH, 2, S5_GROUPS, S5_GROUP, S5_STATE), 0.25)
    s5_d = nrm((DEPTH, S5_WIDTH), 0.5)
    s5_w_glu = nrm((DEPTH, S5_WIDTH, S5_WIDTH), S5_WIDTH ** -0.5)
    lru_conv_w = nrm((DEPTH, LRU_CONV, LRU_WIDTH), LRU_CONV ** -0.5)
    lru_conv_b = nrm((DEPTH, LRU_WIDTH), 0.01)
    lru_w_a = nrm((DEPTH, 2, LRU_BLOCKS, LRU_BLOCK, LRU_BLOCK), LRU_BLOCK ** -0.5)
    lru_b_a = nrm((DEPTH, 2, LRU_WIDTH), 0.01)
    lru_w_x = nrm((DEPTH, 2, LRU_BLOCKS, LRU_BLOCK, LRU_BLOCK), LRU_BLOCK ** -0.5)
    lru_b_x = nrm((DEPTH, 2, LRU_WIDTH), 0.01)
    a0 = unif((DEPTH, 2, LRU_WIDTH), 0.9, 0.999)
    sig = a0 ** (1.0 / LRU_C)
    lru_lam = jnp.log(sig) - jnp.log1p(-sig)
    ssd_conv_w = nrm((DEPTH, SSD_CONV, SSD_XBC), SSD_CONV ** -0.5)
    ssd_conv_b = nrm((DEPTH, SSD_XBC), 0.01)
    ssd_a_log = jnp.log(unif((DEPTH, 2, SSD_HEADS), 1.0, 16.0))
    dt0 = jnp.exp(unif((DEPTH, 2, SSD_HEADS), math.log(1e-3), math.log(1e-1)))
    ssd_dt_bias = dt0 + jnp.log(-jnp.expm1(-dt0))
    ssd_d = 1.0 + nrm((DEPTH, SSD_HEADS), 0.1)
    ssd_norm_w = 1.0 + nrm((DEPTH, SSD_INNER), 0.05)
    w_br_a = nrm((DEPTH, S5_WIDTH, D_MODEL), S5_WIDTH ** -0.5)
    w_br_b = nrm((DEPTH, LRU_WIDTH, D_MODEL), LRU_WIDTH ** -0.5)
    w_br_c = nrm((DEPTH, SSD_INNER, D_MODEL), SSD_INNER ** -0.5)
    w_out = nrm((DEPTH, D_MODEL, D_MODEL), D_MODEL ** -0.5)
    ffn_w1 = nrm((N_DENSE, D_MODEL, D_FF), D_MODEL ** -0.5)
    ffn_w3 = nrm((N_DENSE, D_MODEL, D_FF), D_MODEL ** -0.5)
    ffn_w2 = nrm((N_DENSE, D_FF, D_MODEL), D_FF ** -0.5)
    moe_w_router = nrm((N_MOE, D_MODEL, N_EXPERTS), D_MODEL ** -0.5)
    moe_b_router = nrm((N_MOE, N_EXPERTS), 0.01)
    moe_w1 = nrm((N_MOE, N_EXPERTS, D_MODEL, D_FF_EXPERT), D_MODEL ** -0.5)
    moe_w3 = nrm((N_MOE, N_EXPERTS, D_MODEL, D_FF_EXPERT), D_MODEL ** -0.5)
    moe_w2 = nrm((N_MOE, N_EXPERTS, D_FF_EXPERT, D_MODEL), D_FF_EXPERT ** -0.5)
    final_norm_w = 1.0 + nrm((D_MODEL,), 0.05)
    return {'x': x, 'c': c, 'ctx': ctx, 'c_ctx': c_ctx, 'w_mod': w_mod, 'b_mod': b_mod, 'norm_w': norm_w,
            'w_in': w_in, 's5_lam_re': s5_lam_re, 's5_lam_im': s5_lam_im, 's5_log_dt': s5_log_dt,
            's5_b_re': s5_b_re, 's5_b_im': s5_b_im, 's5_c_re': s5_c_re, 's5_c_im': s5_c_im, 's5_d': s5_d,
            's5_w_glu': s5_w_glu, 'lru_conv_w': lru_conv_w, 'lru_conv_b': lru_conv_b, 'lru_w_a': lru_w_a,
            'lru_b_a': lru_b_a, 'lru_w_x': lru_w_x, 'lru_b_x': lru_b_x, 'lru_lam': lru_lam,
            'ssd_conv_w': ssd_conv_w, 'ssd_conv_b': ssd_conv_b, 'ssd_a_log': ssd_a_log, 'ssd_dt_bias': ssd_dt_bias,
            'ssd_d': ssd_d, 'ssd_norm_w': ssd_norm_w, 'w_br_a': w_br_a, 'w_br_b': w_br_b, 'w_br_c': w_br_c,
            'w_out': w_out, 'ffn_w1': ffn_w1, 'ffn_w3': ffn_w3, 'ffn_w2': ffn_w2, 'moe_w_router': moe_w_router,
            'moe_b_router': moe_b_router, 'moe_w1': moe_w1, 'moe_w3': moe_w3, 'moe_w2': moe_w2,
            'final_norm_w': final_norm_w}


def reference(x, c, ctx, c_ctx, w_mod, b_mod, norm_w, w_in, s5_lam_re, s5_lam_im, s5_log_dt, s5_b_re, s5_b_im,
              s5_c_re, s5_c_im, s5_d, s5_w_glu, lru_conv_w, lru_conv_b, lru_w_a, lru_b_a, lru_w_x, lru_b_x, lru_lam,
              ssd_conv_w, ssd_conv_b, ssd_a_log, ssd_dt_bias, ssd_d, ssd_norm_w, w_br_a, w_br_b, w_br_c, w_out,
              ffn_w1, ffn_w3, ffn_w2, moe_w_router, moe_b_router, moe_w1, moe_w3, moe_w2, final_norm_w):
    cond_lat = jax.nn.silu(c)
    cond_ctx = jax.nn.silu(c_ctx)
    x_lat, x_ctx = x, ctx
    n_ctx = ctx.shape[1]
    for l in range(DEPTH):
        ctx_out = l < DEPTH - 1
        m_lat = jnp.split((cond_lat @ w_mod[l] + b_mod[l])[:, None, :], N_MOD, axis=-1)
        m_ctx = jnp.split(cond_ctx @ w_mod[l] + b_mod[l], N_MOD, axis=-1)
        h_lat = modulate(rmsnorm(x_lat, norm_w[l, 0]), m_lat[0], m_lat[1])
        h_ctx = modulate(rmsnorm(x_ctx, norm_w[l, 0]), m_ctx[0], m_ctx[1])
        y_lat, y_ctx = token_mixer(
            h_ctx, h_lat, w_in[l], s5_lam_re[l], s5_lam_im[l], s5_log_dt[l], s5_b_re[l], s5_b_im[l], s5_c_re[l],
            s5_c_im[l], s5_d[l], s5_w_glu[l], lru_conv_w[l], lru_conv_b[l], lru_w_a[l], lru_b_a[l], lru_w_x[l],
            lru_b_x[l], lru_lam[l], ssd_conv_w[l], ssd_conv_b[l], ssd_a_log[l], ssd_dt_bias[l], ssd_d[l],
            ssd_norm_w[l], w_br_a[l], w_br_b[l], w_br_c[l], w_out[l], ctx_out)
        x_lat = x_lat + m_lat[2] * y_lat
        h_lat = modulate(rmsnorm(x_lat, norm_w[l, 1]), m_lat[3], m_lat[4])
        if ctx_out:
            x_ctx = x_ctx + m_ctx[2] * y_ctx
            h_ctx = modulate(rmsnorm(x_ctx, norm_w[l, 1]), m_ctx[3], m_ctx[4])
            h_all = jnp.concatenate([h_ctx, h_lat], axis=1)
        else:
            h_all = h_lat
        if l % 2 == 0:
            f = swiglu(h_all, ffn_w1[l // 2], ffn_w3[l // 2], ffn_w2[l // 2])
        else:
            f = moe_swiglu(h_all, moe_w_router[l // 2], moe_b_router[l // 2], moe_w1[l // 2], moe_w3[l // 2],
                           moe_w2[l // 2])
        if ctx_out:
            x_ctx = x_ctx + m_ctx[5] * f[:, :n_ctx]
            x_lat = x_lat + m_lat[5] * f[:, n_ctx:]
        else:
            x_lat = x_lat + m_lat[5] * f
    return rmsnorm(x_lat, final_norm_w)
```

```python
import math
import os
from contextlib import ExitStack
import numpy as np
import concourse.bass as bass
import concourse.mybir as mybir
from concourse.bass_utils import run_bass_kernel_spmd

F32 = mybir.dt.float32
F32R = mybir.dt.float32r
I32 = mybir.dt.int32
AF = mybir.ActivationFunctionType
ALU = mybir.AluOpType
AX = mybir.AxisListType

SAME_ENGINE_SYNC = True
EPS = 1e-6
D = 1024
GROUPS = [[0, 1, 2, 3], [4, 5, 6, 7]]


def R(ap):
    return ap.bitcast(F32R)


class Buf:
    def __init__(self, ap, name=""):
        self.ap = ap
        self.name = name
        self.w = None
        self.r = []
        self.dram = False
        self.wall = {}
        self.rd = []
        self.excl = None
        self.dsem = None

    def __getitem__(self, idx):
        return self.ap[idx]

    def views(self, n):
        return [Buf(self.ap[:, i, :], f"{self.name}_{i}") for i in range(n)]


class K:
    def __init__(self, nc):
        self.nc = nc
        self.engs = {}
        for name, h in (("pe", nc.tensor), ("dve", nc.vector), ("act", nc.scalar),
                        ("pool", nc.gpsimd), ("sp", nc.sync)):
            sem = nc.alloc_semaphore("s_" + name)
            self.engs[name] = dict(h=h, sem=sem, n=0, seen={}, name=name)
        self.sem_pool = []
        self.semval = {}
        self.phase_sems = []
        self.drams = []
        self.stack = None
        self.pname = ""
        self.nbuf = 0

    def phase_begin(self, name):
        self.stack = ExitStack()
        self.pname = name
        self.phase_sems = []

    def phase_end(self):
        toks = [(e["sem"], e["n"], e["name"] + "_bar") for e in self.engs.values()]
        toks += [(s, self.semval[id(s)], "dma") for s in self.phase_sems]
        for e in self.engs.values():
            self._wait(e, toks)
        self.stack.close()
        self.stack = None
        self.sem_pool.extend(self.phase_sems)
        self.phase_sems = []
        for b in self.drams:
            b.wall = {}
            b.rd = []
            b.dsem = None

    def renew_engine_sems(self):
        for name, e in self.engs.items():
            self.nbuf += 1
            self.old_sems = getattr(self, "old_sems", []) + [e["sem"]]
            e["sem"] = self.nc.alloc_semaphore(f"s_{name}_r{self.nbuf}")
            e["n"] = 0

    def _sem(self):
        if self.sem_pool:
            s = self.sem_pool.pop()
        else:
            s = self.nc.alloc_semaphore(f"d{len(self.semval)}")
            self.semval[id(s)] = 0
        self.phase_sems.append(s)
        return s

    def sb(self, shape, dtype=F32, name=None):
        self.nbuf += 1
        name = f"{self.pname}_{name or 'sb'}_{self.nbuf}"
        t = self.stack.enter_context(self.nc.sbuf_tensor(name, list(shape), dtype))
        return Buf(t.ap(), name)

    def ps(self, shape, dtype=F32, name=None):
        self.nbuf += 1
        name = f"{self.pname}_{name or 'ps'}_{self.nbuf}"
        t = self.stack.enter_context(self.nc.psum_tensor(name, list(shape), dtype))
        b = Buf(t.ap(), name)
        b.excl = Buf(None, name + "_bank")
        return b

    def psview(self, parent, ap, name):
        b = Buf(ap, name)
        b.excl = parent.excl
        return b

    def dram(self, name, shape, dtype=F32, kind="Internal"):
        t = self.nc.dram_tensor(name, list(shape), dtype, kind=kind)
        b = Buf(t.ap(), name)
        b.dram = True
        self.drams.append(b)
        return b

    def _wait(self, e, toks):
        need = {}
        for tok in toks:
            if tok is None:
                continue
            sem, val, en = tok
            if en == e["name"] and (not SAME_ENGINE_SYNC or en in ("pe", "sp")):
                continue
            if en == e["name"] + "_bar":
                continue
            kk = id(sem)
            if need.get(kk, (None, 0))[1] < val:
                need[kk] = (sem, val)
        for kk, (sem, val) in need.items():
            if e["seen"].get(kk, 0) < val:
                e["h"].wait_ge(sem, val)
                e["seen"][kk] = val

    def _deps(self, reads, writes):
        toks = []
        for b in reads:
            toks.append(b.w)
        for b in writes:
            toks.append(b.w)
            toks.extend(b.r)
        return toks

    def op(self, eng, fn, reads=(), writes=()):
        e = self.engs[eng]
        reads = list(reads)
        writes = list(writes)
        for b in reads + writes:
            if b.excl is not None and b.excl not in writes:
                writes.append(b.excl)
        self._wait(e, self._deps(reads, writes))
        e["n"] += 1
        fn(e["h"]).then_inc(e["sem"], 1)
        tok = (e["sem"], e["n"], eng)
        for b in reads:
            b.r.append(tok)
        for b in writes:
            b.w = tok
            b.r = []

    def dma(self, outs, out_ap, ins, in_ap, q="sp", **kw):
        e = self.engs[q]
        outs = [] if outs is None else (outs if isinstance(outs, (list, tuple)) else [outs])
        ins = [] if ins is None else (ins if isinstance(ins, (list, tuple)) else [ins])
        sb_in = [b for b in ins if not b.dram]
        owner = sb_in[0] if sb_in else outs[0]
        if owner.dsem is None:
            owner.dsem = self._sem()
        toks = []
        for b in ins:
            if b.dram:
                toks.extend(b.wall.values())
            else:
                toks.append(b.w)
        for b in outs:
            if b.dram:
                toks.extend(b.rd)
            else:
                if b.w is not None and b.w[0] is not owner.dsem:
                    toks.append(b.w)
                toks.extend(b.r)
        self._wait(e, toks)
        self.semval[id(owner.dsem)] += 16
        e["h"].dma_start(out=out_ap, in_=in_ap, **kw).then_inc(owner.dsem, 16)
        tok = (owner.dsem, self.semval[id(owner.dsem)], "dma")
        for b in sb_in:
            b.r.append(tok)
        for b in ins:
            if b.dram and getattr(b, "track", False):
                b.rd.append(tok)
        for b in outs:
            if b.dram:
                b.wall[id(owner.dsem)] = tok
            else:
                b.w = tok
                b.r = []

    def collective(self, kind, in_b, out_b, groups, sem=None):
        e = self.engs["pool"]
        toks = list(in_b.wall.values()) + list(out_b.wall.values()) + list(out_b.rd)
        self._wait(e, toks)
        sem = sem or self._sem()
        self.semval[id(sem)] += 1
        e["h"].collective_compute(kind, ALU.bypass, replica_groups=groups, ins=[in_b.ap.opt()],
                                  outs=[out_b.ap.opt()]).then_inc(sem, 1)
        tok = (sem, self.semval[id(sem)], "dma")
        out_b.wall[id(sem)] = tok
        in_b.rd.append(tok)

    def cc_sem(self, name):
        sem = self.nc.alloc_semaphore(name)
        self.semval[id(sem)] = 0
        self.keep = getattr(self, "keep", []) + [sem]
        return sem

    def gather_async(self, pieces, sem, toks=()):
        self._wait(self.engs["pool"], list(toks))
        for (src_ap, dst_ap) in pieces:
            sb_ = Buf(src_ap, "gsrc"); sb_.dram = True
            db_ = Buf(dst_ap, "gdst"); db_.dram = True
            self.collective("AllGather", sb_, db_, GROUPS, sem=sem)

    def cc_wait(self, sem):
        tok = (sem, self.semval[id(sem)], "dma")
        for e in self.engs.values():
            self._wait(e, [tok])

    def finish(self, bufs, eng="sp"):
        e = self.engs[eng]
        toks = []
        for b in bufs:
            toks.append(b.w)
            toks.extend(b.r)
            toks.extend(b.wall.values())
        self._wait(e, toks)


def nsplit(W):
    return [(0, min(W, 512))] + ([(512, W)] if W > 512 else [])


def linear(k, wd, mcs, KC, rhs, W, evac):
    for mc in mcs:
        wb = k.wbuf[k.wi % 2]
        k.wi += 1
        for k0 in range(0, KC, 8):
            n = min(8, KC - k0)
            st = k.wstg[k.si % len(k.wstg)]
            k.si += 1
            k.dma(st, st[:, 0:n, :], wd, wd.ap[mc][:, k0:k0 + n, :], q="sp")
            k.op("dve", lambda e, st=st, wb=wb, k0=k0, n=n: e.tensor_copy(out=wb[:, k0:k0 + n, :], in_=st[:, 0:n, :]),
                 reads=[st], writes=[wb])
        ps = k.psl[k.pi % len(k.psl)]
        k.pi += 1
        for (n0, n1) in nsplit(W):
            for kc in range(KC):
                rb = rhs[kc]
                k.op("pe", lambda e, ps=ps, wb=wb, rb=rb, kc=kc, n0=n0, n1=n1: e.matmul(
                    ps[:, n0:n1], wb[:, kc, :], R(rb[:, n0:n1]), start=(kc == 0), stop=(kc == KC - 1)),
                    reads=[wb, rb], writes=[ps])
        evac(mc, ps)


def load_h(k, hall, hv, hb, ch, off=0, zt=None):
    def src(r, bl, pg, c0, n):
        return hall.ap[bl, pg, r, :, :, c0:c0 + n]
    if ch == 0:
        for bl in range(4):
            c0 = off + bl * 64
            for pg in range(4):
                k.dma(hv, R(hb[32 * pg:32 * pg + 32, :, c0:c0 + 64]), hall, src(0, bl, pg, 512, 64), q="pool")
    else:
        t0 = (ch - 1) * 256
        r, bl, col = t0 // 2048, (t0 % 2048) // 512, t0 % 512
        for pg in range(4):
            k.dma(hv, R(hb[32 * pg:32 * pg + 32, :, off:off + 256]), hall, src(r, bl, pg, col, 256), q="pool")
    if zt is not None:
        for side in (0, 1):
            dst = hb[:, :, 0:2] if side == 0 else hb[:, :, off + 256:off + 258]
            tt = (ch - 1) * 256 - 2 if side == 0 else ch * 256
            if ch == 0 or tt < 0 or tt >= 8192:
                k.op("act", lambda e, dst=dst: e.activation(out=R(dst), in_=zt[:, :, 0:2], func=AF.Copy), reads=[zt], writes=hv)
            else:
                r, bl, col = tt // 2048, (tt % 2048) // 512, tt % 512
                d0 = 0 if side == 0 else off + 256
                for pg in range(4):
                    k.dma(hv, R(hb[32 * pg:32 * pg + 32, :, d0:d0 + 2]), hall, src(r, bl, pg, col, 2), q="pool")


def rmsnorm(k, X, W, segs, scale_t, shift_t, out, f32r, sqs, ones, shift_b=None):
    ps = k.psl[k.pi % len(k.psl)]
    k.pi += 1
    for c in range(8):
        sq = sqs[c % 2]
        k.op("act", lambda e, sq=sq, c=c: e.activation(out=R(sq[:, 0:W]), in_=X[c][:, 0:W], func=AF.Square),
             reads=[X[c]], writes=[sq])
        for (n0, n1) in nsplit(W):
            k.op("pe", lambda e, sq=sq, c=c, n0=n0, n1=n1: e.matmul(
                ps[:, n0:n1], ones[:, :], R(sq[:, n0:n1]), start=(c == 0), stop=(c == 7)),
                reads=[sq, ones], writes=[ps])
    rstd = k.rstd
    k.op("act", lambda e: e.activation(out=rstd[:, 0:W], in_=ps[:, 0:W], func=AF.Sqrt, scale=1.0 / D, bias=k.epsb[:, 0:1]),
         reads=[ps, k.epsb], writes=[rstd])
    k.op("dve", lambda e: e.reciprocal(out=rstd[:, 0:W], in_=rstd[:, 0:W]), reads=[rstd], writes=[rstd])
    for c in range(8):
        for (c0, c1, j) in segs:
            o = out[c][:, c0:c1]
            if f32r:
                o = R(o)
            if shift_t is None:
                k.op("dve", lambda e, o=o, c=c, c0=c0, c1=c1, j=j: e.scalar_tensor_tensor(
                    out=o, in0=X[c][:, c0:c1], scalar=scale_t[:, c, j:j + 1], in1=rstd[:, c0:c1],
                    op0=ALU.mult, op1=ALU.mult), reads=[X[c], scale_t, rstd], writes=[out[c]])
            else:
                tmp = k.tmp[c % 2]
                k.op("dve", lambda e, tmp=tmp, c=c, c0=c0, c1=c1, j=j: e.scalar_tensor_tensor(
                    out=tmp[:, c0:c1], in0=X[c][:, c0:c1], scalar=scale_t[:, c, j:j + 1], in1=rstd[:, c0:c1],
                    op0=ALU.mult, op1=ALU.mult), reads=[X[c], scale_t, rstd], writes=[tmp])
                k.op("act", lambda e, o=o, tmp=tmp, c=c, c0=c0, c1=c1, j=j: e.activation(
                    out=o, in_=tmp[:, c0:c1], func=AF.Identity, bias=shift_t[:, c, j:j + 1], scale=1.0),
                    reads=[tmp, shift_b], writes=[out[c]])


def phase_C(k, P, has_mix, ffn, has_ctx, tail, xsrc, hsrc, yall, qv, hdst, xdst, oT, ystage=None, hgather=None, NB=4):
    k.phase_begin("C")
    W = 576 if has_ctx else 512
    segs = [(0, 512, 0)] + ([(512, 576, 1)] if has_ctx else [])
    xT = xsrc
    if has_mix:
        hT = hsrc
        w_z, w_g, w_glu = P["w_z"], P["w_g"], P["w_glu"]
        w_bra, w_brb, w_brc, w_out, ssd_nw = P["w_bra"], P["w_brb"], P["w_brc"], P["w_out"], P["ssd_nw"]
    if has_mix or ffn:
        w_mod, b_mod, nw = P["w_mod"], P["b_mod"], P["nw"]
    cond = P["cond"]
    if ffn == "dense":
        w1, w3, w2 = P["w1"], P["w3"], P["w2"]
        NH = 22
    elif ffn == "moe":
        w1, w3, w2 = P["w1"], P["w3"], P["w2"]
        wr, br, sel, ident = P["wr"], P["br"], P["sel"], P["ident"]
        NH = 28
    else:
        NH = 0
    if tail == "h_next":
        w_modn, b_modn, nwn = P["w_modn"], P["b_modn"], P["nwn"]
        hT_o, xT_o = hdst, xdst
    else:
        fnw = P["fnw"]

    k.wbuf = [k.sb([128, 28, 128], F32R, name=f"wb{i}") for i in range(2)]
    k.wstg = [k.sb([128, 8, 128], name=f"wstg{i}") for i in range(4)]
    k.si = 0
    k.wi = 0
    k.psl = [k.ps([128, 1024], name=f"psl{i}") for i in range(3)]
    k.pi = 0
    psx = k.ps([128, 1024], name="psx")
    Xt = k.sb([128, 8, W], name="X"); X = Xt.views(8)
    Ht = k.sb([128, 8, W], name="H"); H = Ht.views(8)
    k.rstd = k.sb([128, W], name="rstd")
    k.tmp = [k.sb([128, W], name=f"tmp{i}") for i in range(2)]
    sqs = [k.sb([128, W], name=f"sq{i}") for i in range(2)]
    k.acc = k.sb([128, W], name="accs")
    OUTt, OUT = Xt, X
    ones32 = k.sb([128, 128], name="ones32")
    k.op("dve", lambda e: e.memset(ones32[:, :], 1.0), writes=[ones32])
    ones = k.sb([128, 128], F32R, name="ones")
    k.op("act", lambda e: e.activation(out=ones[:, :], in_=ones32[:, :], func=AF.Copy), reads=[ones32], writes=[ones])
    k.epsb = k.sb([128, 1], name="epsb")
    k.op("dve", lambda e: e.memset(k.epsb[:, :], EPS), writes=[k.epsb])
    if has_mix or ffn:
        MGt = k.sb([128, 8, W], name="MG"); MG = MGt.views(8)
        St = k.sb([128, max(30, NH), W], name="S"); S = St.views(max(30, NH))
        YG, YB, YC, Z = S[0:6], S[6:14], S[14:22], S[22:30]
        HID = S

    condt = k.sb([128, 8, 2], name="condt")
    k.dma(condt, condt[:], cond, cond[:])
    conds_t = k.sb([128, 8, 2], name="conds"); conds = conds_t.views(8)
    for c in range(8):
        k.op("act", lambda e, c=c: e.activation(out=R(conds[c][:, :]), in_=condt[:, c, :], func=AF.Silu),
             reads=[condt], writes=[conds[c]])

    def mod_vectors(wd, bd, nmc, name):
        bt = k.sb([128, nmc], name=name + "_b")
        k.dma(bt, bt[:], bd, bd[:])
        mt = k.sb([128, nmc, 2], name=name)

        def ev(mc, ps):
            k.op("dve", lambda e, mc=mc, ps=ps: e.tensor_scalar(out=mt[:, mc, :], in0=ps[:, 0:2], scalar1=bt[:, mc:mc + 1],
                                                               scalar2=None, op0=ALU.add), reads=[ps, bt], writes=[mt])
        linear(k, wd, range(nmc), 8, conds, 2, ev)
        return mt

    def scale_of(mt, off, nwt, nwi, name):
        st = k.sb([128, 8, 2], name=name)
        for j in range(2):
            k.op("dve", lambda e, j=j: e.scalar_tensor_tensor(out=st[:, :, j], in0=mt[:, off:off + 8, j], scalar=1.0,
                                                              in1=nwi, op0=ALU.add, op1=ALU.mult),
                 reads=[mt, nwt], writes=[st])
        return st

    if has_mix or ffn:
        MOD = mod_vectors(w_mod, b_mod, 48, "MOD")
        nwt = k.sb([128, 2, 8], name="nwt")
        k.dma(nwt, nwt[:], nw, nw[:])
        A4 = scale_of(MOD, 32, nwt, nwt[:, 1, :], "A4")
    if tail == "h_next":
        MODN = mod_vectors(w_modn, b_modn, 16, "MODN")
        nwnt = k.sb([128, 8], name="nwnt")
        k.dma(nwnt, nwnt[:], nwn, nwn[:])
        A1N = scale_of(MODN, 8, nwnt, nwnt[:, :], "A1N")
    else:
        fnwt = k.sb([128, 8, 1], name="fnwt")
        k.dma(fnwt, fnwt[:, :, 0], fnw, fnw[:])
    if has_mix:
        snw = k.sb([128, 8, 1], name="snw")
        k.dma(snw, snw[:, :, 0], ssd_nw, ssd_nw[:])
    if ffn == "moe":
        wrt = k.sb([128, 8, 8], F32R, name="wrt")
        k.dma(wrt, wrt[:], wr, wr[:], q="pool")
        brt = k.sb([128, 8], name="brt")
        k.dma(brt, brt[:], br, br[:])
        selt = k.sb([8, 8, 128], F32R, name="selt")
        k.dma(selt, selt[:], sel, sel[:], q="pool")
        idt = k.sb([128, 128], name="idt")
        k.dma(idt, idt[:], ident, ident[:])
        GT = k.sb([8, W], name="GT")
        GBt = k.sb([128, 8, W], name="GB"); GB = GBt.views(8)
        L = k.sb([128, 8], name="L"); M8 = k.sb([128, 8], name="M8"); msk = k.sb([128, 8], name="msk")
        ex = k.sb([128, 8], name="ex"); sc1 = k.sb([128, 2], name="sc1")

    def resid(mc, ps, moff):
        for (c0, c1, j) in segs:
            k.op("dve", lambda e, c0=c0, c1=c1, j=j: e.scalar_tensor_tensor(
                out=X[mc][:, c0:c1], in0=ps[:, c0:c1], scalar=MOD[:, moff + mc, j:j + 1], in1=X[mc][:, c0:c1],
                op0=ALU.mult, op1=ALU.add), reads=[ps, MOD, X[mc]], writes=[X[mc]])

    for blk in range(NB):
        k.dma(X, Xt[:], xT, xT.ap[blk][:, :, 0:W])
        if has_mix:
            k.dma(H, R(Ht[:]), hT, hT.ap[blk][:, :, 0:W], q="pool")
            tb = qv * 4 + blk
            yst = ystage[blk % 2]
            k.dma(yst, yst.ap.rearrange("m r p c w -> m (r p c w)"), yall,
                  yall.ap[:, bass.ds(tb, 1)].rearrange("m o r p c w -> m (o r p c w)"), q="pool")
            def ld_y(dst_bufs, dst_t, c_dst, p0, np_, r, m, c_src, ps0):
                k.dma(dst_bufs, R(dst_t[p0:p0 + np_, c_dst, 0:512]), yst, yst.ap[m, r, ps0:ps0 + np_, c_src, :], q="pool")
                if has_ctx:
                    k.dma(dst_bufs, R(dst_t[p0:p0 + np_, c_dst, 512:576]), yall, yall.ap[m, 16, r, ps0:ps0 + np_, c_src, blk * 64:blk * 64 + 64], q="pool")
            for gt_ in range(6):
                g0 = gt_ * 128
                while g0 < gt_ * 128 + 128:
                    r = g0 // 192
                    lc = g0 - 192 * r
                    c_src, ps0 = lc // 128, lc % 128
                    n_ = min(128 - ps0, gt_ * 128 + 128 - g0, 192 - lc)
                    ld_y([YG[gt_]], St, gt_, g0 - gt_ * 128, n_, r, 0, c_src, ps0)
                    g0 += n_
            for gt_ in range(8):
                ld_y([YB[gt_]], St, 6 + gt_, 0, 128, gt_ // 2, 1, gt_ % 2, 0)
                ld_y([YC[gt_]], St, 14 + gt_, 0, 128, gt_ // 2, 2, gt_ % 2, 0)
            if os.environ.get("KF_DEBUG") and blk == 1:
                dS = k.dram("dbg_S" if has_ctx else "dbg_S1", [128, 22, W], kind="ExternalOutput")
                k.dma(dS, dS.ap, S[0:22], St[:, 0:22, :], q="act")
                dX = k.dram("dbg_X" if has_ctx else "dbg_X1", [128, 16, W], kind="ExternalOutput")
                k.dma(dX, dX.ap[:, 0:8, :], X, Xt[:], q="act")
                k.dma(dX, dX.ap[:, 8:16, :], H, Ht[:], q="act")
            def ev_glu(mc, ps):
                t = k.tmp[mc % 2]
                k.op("act", lambda e, t=t, ps=ps: e.activation(out=t[:, 0:W], in_=ps[:, 0:W], func=AF.Sigmoid),
                     reads=[ps], writes=[t])
                k.op("dve", lambda e, t=t, mc=mc: e.tensor_tensor(out=R(MG[mc][:, 0:W]), in0=t[:, 0:W],
                                                                   in1=YG[mc][:, 0:W].bitcast(F32), op=ALU.mult),
                     reads=[t, YG[mc]], writes=[MG[mc]])
            linear(k, w_glu, range(6), 6, YG, W, ev_glu)
            YA = MG[0:6]
            def ev_z(mc, ps):
                t = k.tmp[mc % 2]
                k.op("act", lambda e, t=t, ps=ps: e.activation(out=t[:, 0:W], in_=ps[:, 0:W], func=AF.Silu),
                     reads=[ps], writes=[t])
                k.op("dve", lambda e, t=t, mc=mc: e.tensor_tensor(out=R(YC[mc][:, 0:W]), in0=t[:, 0:W], in1=YC[mc][:, 0:W],
                                                                   op=ALU.mult), reads=[t, YC[mc]], writes=[YC[mc]])
            linear(k, w_z, range(8), 8, H, W, ev_z)
            rmsnorm(k, YC, W, [(0, W, 0)], snw, None, Z, True, sqs, ones)
            YCN = Z
            MRG = YC
            for mc in range(8):
                first = [True]
                for bi, (wbr, KCb, src) in enumerate(((w_bra, 6, YA), (w_brb, 8, YB), (w_brc, 8, YCN))):
                    gt = k.tmp[bi % 2]

                    def ev_gate(_mc, ps, gt=gt):
                        k.op("act", lambda e, ps=ps, gt=gt: e.activation(out=gt[:, 0:W], in_=ps[:, 0:W], func=AF.Sigmoid),
                             reads=[ps], writes=[gt])
                    linear(k, w_g, [bi * 8 + mc], 8, H, W, ev_gate)

                    def ev_br(_mc, ps, gt=gt, bi=bi, mc=mc):
                        if bi == 0:
                            k.op("dve", lambda e, ps=ps, gt=gt, mc=mc: e.tensor_tensor(
                                out=k.acc[:, 0:W], in0=ps[:, 0:W], in1=gt[:, 0:W], op=ALU.mult),
                                reads=[ps, gt], writes=[k.acc])
                        else:
                            k.op("dve", lambda e, ps=ps, gt=gt, mc=mc: e.tensor_tensor(
                                out=gt[:, 0:W], in0=ps[:, 0:W], in1=gt[:, 0:W], op=ALU.mult),
                                reads=[ps, gt], writes=[gt])
                            o = R(MRG[mc][:, 0:W]) if bi == 2 else k.acc[:, 0:W]
                            ob = MRG[mc] if bi == 2 else k.acc
                            k.op("dve", lambda e, gt=gt, o=o: e.tensor_tensor(
                                out=o, in0=gt[:, 0:W], in1=k.acc[:, 0:W], op=ALU.add),
                                reads=[gt, k.acc], writes=[ob])
                    linear(k, wbr, [mc], KCb, src, W, ev_br)
            linear(k, w_out, range(8), 8, MRG, W, lambda mc, ps: resid(mc, ps, 16))
        if ffn:
            rmsnorm(k, X, W, segs, A4, MOD[:, 24:32, :], H, True, sqs, ones, shift_b=MOD)
            if ffn == "dense":
                experts = [0]
            else:
                experts = list(range(8))
                for sb_ in range(W // 128):
                    t0 = sb_ * 128
                    for kc in range(8):
                        k.op("pe", lambda e, kc=kc, t0=t0: e.matmul(psx[:, 0:8], R(H[kc][:, t0:t0 + 128]), wrt[:, kc, :],
                                                                    start=(kc == 0), stop=(kc == 7)),
                             reads=[H[kc], wrt], writes=[psx])
                    k.op("dve", lambda e: e.tensor_tensor(out=L[:, :], in0=psx[:, 0:8], in1=brt[:, :], op=ALU.add),
                         reads=[psx, brt], writes=[L])
                    k.op("dve", lambda e: e.max(out=M8[:, :], in_=L[:, :]), reads=[L], writes=[M8])
                    k.op("dve", lambda e: e.tensor_scalar(out=msk[:, :], in0=L[:, :], scalar1=M8[:, 1:2], scalar2=None,
                                                          op0=ALU.is_ge), reads=[L, M8], writes=[msk])
                    k.op("dve", lambda e: e.tensor_scalar(out=sc1[:, 0:1], in0=M8[:, 0:1], scalar1=-1.0, scalar2=None,
                                                          op0=ALU.mult), reads=[M8], writes=[sc1])
                    k.op("act", lambda e: e.activation(out=ex[:, :], in_=L[:, :], func=AF.Exp, bias=sc1[:, 0:1], scale=1.0),
                         reads=[L, sc1], writes=[ex])
                    k.op("dve", lambda e: e.tensor_tensor(out=ex[:, :], in0=ex[:, :], in1=msk[:, :], op=ALU.mult),
                         reads=[ex, msk], writes=[ex])
                    k.op("dve", lambda e: e.reduce_sum(out=sc1[:, 1:2], in_=ex[:, :], axis=AX.X), reads=[ex], writes=[sc1])
                    k.op("dve", lambda e: e.reciprocal(out=sc1[:, 1:2], in_=sc1[:, 1:2]), reads=[sc1], writes=[sc1])
                    k.op("dve", lambda e: e.tensor_scalar(out=ex[:, :], in0=ex[:, :], scalar1=sc1[:, 1:2], scalar2=None,
                                                          op0=ALU.mult), reads=[ex, sc1], writes=[ex])
                    k.op("pe", lambda e: e.transpose(out=psx[0:8, 512:640], in_=ex[:, :], identity=idt[:, :]),
                         reads=[ex, idt], writes=[psx])
                    k.op("act", lambda e, t0=t0: e.activation(out=R(GT[:, t0:t0 + 128]), in_=psx[0:8, 512:640], func=AF.Copy),
                         reads=[psx], writes=[GT])
                for ex_i in range(8):
                    k.op("pe", lambda e, ex_i=ex_i: e.matmul(psx[:, 0:W], selt[:, ex_i, :], R(GT[:, 0:W]), start=True, stop=True),
                         reads=[selt, GT], writes=[psx])
                    k.op("act", lambda e, ex_i=ex_i: e.activation(out=GB[ex_i][:, 0:W], in_=psx[:, 0:W], func=AF.Copy),
                         reads=[psx], writes=[GB[ex_i]])
            for ei in experts:
                for mc in range(NH):
                    t = k.tmp[mc % 2]

                    def ev1(_mc, ps, t=t):
                        k.op("act", lambda e, ps=ps, t=t: e.activation(out=t[:, 0:W], in_=ps[:, 0:W], func=AF.Silu),
                             reads=[ps], writes=[t])
                    linear(k, w1, [ei * NH + mc], 8, H, W, ev1)

                    def ev3(_mc, ps, t=t, mc=mc, ei=ei):
                        if ffn == "dense":
                            k.op("dve", lambda e, ps=ps, t=t, mc=mc: e.tensor_tensor(
                                out=R(HID[mc][:, 0:W]), in0=ps[:, 0:W], in1=t[:, 0:W], op=ALU.mult),
                                reads=[ps, t], writes=[HID[mc]])
                        else:
                            k.op("dve", lambda e, ps=ps, t=t: e.tensor_tensor(
                                out=t[:, 0:W], in0=ps[:, 0:W], in1=t[:, 0:W], op=ALU.mult), reads=[ps, t], writes=[t])
                            k.op("pool", lambda e, t=t, mc=mc, ei=ei: e.tensor_tensor(
                                out=R(HID[mc][:, 0:W]), in0=t[:, 0:W], in1=GB[ei][:, 0:W], op=ALU.mult),
                                reads=[t, GB[ei]], writes=[HID[mc]])
                    linear(k, w3, [ei * NH + mc], 8, H, W, ev3)
                for mc in range(8):
                    linear(k, w2, [ei * 8 + mc], NH, HID, W, lambda _mc, ps, mc=mc: resid(mc, ps, 40))
        if tail == "h_next":
            if xT_o is not None:
                k.dma(xT_o, xT_o.ap[blk][:, :, 0:W], X, Xt[:], q="act")
            rmsnorm(k, X, W, segs, A1N, MODN[:, 0:8, :], OUT, False, sqs, ones, shift_b=MODN)
            k.dma(hT_o, hT_o.ap[blk][:, :, 0:W], OUT, OUTt[:], q="act")
            if hgather is not None:
                hall_, sem_ = hgather
                k.gather_async([(hT_o.ap[blk, 32 * pg_:32 * pg_ + 32], hall_.ap[blk, pg_]) for pg_ in range(4)], sem_,
                               toks=list(hT_o.wall.values()))
        else:
            rmsnorm(k, X, W, [(0, W, 0)], fnwt, None, OUT, False, sqs, ones)
            k.dma(oT, oT[blk], OUT, OUTt[:], q="act")
    k.phase_end()


Q = 256
NCH = 33
TWO_PI = 6.283185
HW_ = 260

def ydst(ymix, m, ch):
    if ch == 0:
        return ymix.ap[m, 16, :, :, 0:256]
    return ymix.ap[m, (ch - 1) // 2, :, :, ((ch - 1) % 2) * 256:((ch - 1) % 2) * 256 + 256]

def phase_S5(k, P, hall, ymix):
    k.phase_begin("S5")
    w_u = P["w_u"]
    lamre, lamim, logdt = P["lamre"], P["lamim"], P["logdt"]
    Bre, Bim, CreT, CimT = P["Bre"], P["Bim"], P["CreT"], P["CimT"]
    dskip, iota, ident = P["dskip"], P["iota"], P["ident"]

    k.wbuf = [k.sb([128, 8, 128], F32R, name=f"wb{i}") for i in range(2)]
    k.wstg = [k.sb([128, 8, 128], name=f"wstg{i}") for i in range(4)]
    k.si = 0
    k.wi = 0
    k.psl = [k.ps([128, 512], name=f"psu{i}") for i in range(2)]
    k.pi = 0
    psb = [k.ps([128, 512], name=f"psb{i}") for i in range(4)]
    psy = [k.ps([128, 512], name=f"psy{i}") for i in range(2)]

    def small(name, shape=(128, 12)):
        return k.sb(list(shape), name=name)

    def ld(name, src, shape, q="sp", dt=F32):
        t = k.sb(list(shape), dt, name=name)
        k.dma(t, t[:], src, src[:], q=q)
        return t

    lre = ld("lre", lamre, [128, 12]); lim = ld("lim", lamim, [128, 12]); ldt = ld("ldt", logdt, [128, 12])
    bre = ld("bre", Bre, [128, 12, 128]); bim = ld("bim", Bim, [128, 12, 128])
    cre = ld("cre", CreT, [128, 12, 128], q="pool", dt=F32R); cim = ld("cim", CimT, [128, 12, 128], q="pool", dt=F32R)
    dsk = ld("dsk", dskip, [128, 2]); iot = ld("iot", iota, [128, Q]); idt = ld("idt", ident, [128, 128])

    V = lambda eng, fn, r, w: k.op(eng, fn, reads=r, writes=w)
    dt_ = small("dt"); rr = small("rr"); thn = small("thn")
    V("act", lambda e: e.activation(out=dt_[:, :], in_=ldt[:, :], func=AF.Exp), [ldt], [dt_])
    V("dve", lambda e: e.tensor_tensor(out=rr[:, :], in0=lre[:, :], in1=dt_[:, :], op=ALU.mult), [lre, dt_], [rr])
    V("act", lambda e: e.activation(out=rr[:, :], in_=rr[:, :], func=AF.Exp), [rr], [rr])
    V("dve", lambda e: e.scalar_tensor_tensor(out=thn[:, :], in0=lim[:, :], scalar=1.0 / (2 * math.pi), in1=dt_[:, :],
                                              op0=ALU.mult, op1=ALU.mult), [lim, dt_], [thn])
    cosT = k.sb([128, 12, Q], name="cosT"); sinT = k.sb([128, 12, Q], name="sinT")
    xf = k.sb([128, Q], name="xf"); xi = k.sb([128, Q], I32, name="xi"); xr = k.sb([128, Q], name="xr")
    mk = k.sb([128, Q], name="mk")

    def wrap(f):
        V("dve", lambda e: e.tensor_single_scalar(out=mk[:, :], in_=f[:, :], scalar=0.5, op=ALU.is_gt), [f], [mk])
        V("dve", lambda e: e.tensor_tensor(out=f[:, :], in0=f[:, :], in1=mk[:, :], op=ALU.subtract), [f, mk], [f])
        V("dve", lambda e: e.tensor_single_scalar(out=mk[:, :], in_=f[:, :], scalar=-0.5, op=ALU.is_lt), [f], [mk])
        V("dve", lambda e: e.tensor_tensor(out=f[:, :], in0=f[:, :], in1=mk[:, :], op=ALU.add), [f, mk], [f])

    for di in range(12):
        V("dve", lambda e, di=di: e.tensor_scalar(out=xf[:, :], in0=iot[:, :], scalar1=thn[:, di:di + 1], scalar2=None,
                                                  op0=ALU.mult), [iot, thn], [xf])
        V("dve", lambda e: e.tensor_copy(out=xi[:, :], in_=xf[:, :]), [xf], [xi])
        V("dve", lambda e: e.tensor_copy(out=xr[:, :], in_=xi[:, :]), [xi], [xr])
        V("dve", lambda e: e.tensor_tensor(out=xf[:, :], in0=xf[:, :], in1=xr[:, :], op=ALU.subtract), [xf, xr], [xf])
        wrap(xf)
        V("act", lambda e, di=di: e.activation(out=sinT[:, di, :], in_=xf[:, :], func=AF.Sin, scale=TWO_PI), [xf], [sinT])
        V("dve", lambda e: e.tensor_single_scalar(out=xf[:, :], in_=xf[:, :], scalar=0.25, op=ALU.add), [xf], [xf])
        wrap(xf)
        V("act", lambda e, di=di: e.activation(out=cosT[:, di, :], in_=xf[:, :], func=AF.Sin, scale=TWO_PI), [xf], [cosT])
    ar = small("ar"); ai = small("ai"); nr = small("nr"); ni = small("ni"); den = small("den"); t12 = small("t12")
    V("dve", lambda e: e.tensor_tensor(out=ar[:, :], in0=rr[:, :], in1=cosT[:, :, 1], op=ALU.mult), [rr, cosT], [ar])
    V("dve", lambda e: e.tensor_single_scalar(out=ar[:, :], in_=ar[:, :], scalar=-1.0, op=ALU.add), [ar], [ar])
    V("dve", lambda e: e.tensor_tensor(out=ai[:, :], in0=rr[:, :], in1=sinT[:, :, 1], op=ALU.mult), [rr, sinT], [ai])
    V("dve", lambda e: e.tensor_tensor(out=nr[:, :], in0=ar[:, :], in1=lre[:, :], op=ALU.mult), [ar, lre], [nr])
    V("dve", lambda e: e.tensor_tensor(out=t12[:, :], in0=ai[:, :], in1=lim[:, :], op=ALU.mult), [ai, lim], [t12])
    V("dve", lambda e: e.tensor_tensor(out=nr[:, :], in0=nr[:, :], in1=t12[:, :], op=ALU.add), [nr, t12], [nr])
    V("dve", lambda e: e.tensor_tensor(out=ni[:, :], in0=ai[:, :], in1=lre[:, :], op=ALU.mult), [ai, lre], [ni])
    V("dve", lambda e: e.tensor_tensor(out=t12[:, :], in0=ar[:, :], in1=lim[:, :], op=ALU.mult), [ar, lim], [t12])
    V("dve", lambda e: e.tensor_tensor(out=ni[:, :], in0=ni[:, :], in1=t12[:, :], op=ALU.subtract), [ni, t12], [ni])
    V("dve", lambda e: e.tensor_tensor(out=den[:, :], in0=lre[:, :], in1=lre[:, :], op=ALU.mult), [lre], [den])
    V("dve", lambda e: e.tensor_tensor(out=t12[:, :], in0=lim[:, :], in1=lim[:, :], op=ALU.mult), [lim], [t12])
    V("dve", lambda e: e.tensor_tensor(out=den[:, :], in0=den[:, :], in1=t12[:, :], op=ALU.add), [den, t12], [den])
    V("dve", lambda e: e.reciprocal(out=den[:, :], in_=den[:, :]), [den], [den])
    V("dve", lambda e: e.tensor_tensor(out=nr[:, :], in0=nr[:, :], in1=den[:, :], op=ALU.mult), [nr, den], [nr])
    V("dve", lambda e: e.tensor_tensor(out=ni[:, :], in0=ni[:, :], in1=den[:, :], op=ALU.mult), [ni, den], [ni])
    breT = k.sb([128, 12, 128], F32R, name="breT"); bimT = k.sb([128, 12, 128], F32R, name="bimT")
    tb1 = k.sb([128, 128], name="tb1"); tb2 = k.sb([128, 128], name="tb2")
    for di in range(12):
        V("dve", lambda e, di=di: e.tensor_scalar(out=tb1[:, :], in0=bim[:, di, :], scalar1=ni[:, di:di + 1], scalar2=None,
                                                  op0=ALU.mult), [bim, ni], [tb1])
        V("dve", lambda e, di=di: e.scalar_tensor_tensor(out=tb1[:, :], in0=bre[:, di, :], scalar=nr[:, di:di + 1],
                                                         in1=tb1[:, :], op0=ALU.mult, op1=ALU.subtract), [bre, nr, tb1], [tb1])
        V("pe", lambda e: e.transpose(out=psb[0][:, 0:128], in_=tb1[:, :], identity=idt[:, :]), [tb1, idt], [psb[0]])
        V("act", lambda e, di=di: e.activation(out=breT[:, di, :], in_=psb[0][:, 0:128], func=AF.Copy), [psb[0]], [breT])
        V("dve", lambda e, di=di: e.tensor_scalar(out=tb2[:, :], in0=bre[:, di, :], scalar1=ni[:, di:di + 1], scalar2=None,
                                                  op0=ALU.mult), [bre, ni], [tb2])
        V("dve", lambda e, di=di: e.scalar_tensor_tensor(out=tb2[:, :], in0=bim[:, di, :], scalar=nr[:, di:di + 1],
                                                         in1=tb2[:, :], op0=ALU.mult, op1=ALU.add), [bim, nr, tb2], [tb2])
        V("pe", lambda e: e.transpose(out=psb[1][:, 0:128], in_=tb2[:, :], identity=idt[:, :]), [tb2, idt], [psb[1]])
        V("act", lambda e, di=di: e.activation(out=bimT[:, di, :], in_=psb[1][:, 0:128], func=AF.Copy), [psb[1]], [bimT])

    YA = k.sb([128, 2, NCH * Q], name="YA")
    YAv = [[Buf(YA.ap[:, c, ch * Q:(ch + 1) * Q], f"YA{c}_{ch}") for ch in range(NCH)] for c in range(2)]
    hbuf = [k.sb([128, 8, Q], name=f"hb{i}") for i in range(2)]
    hvs = [hb.views(8) for hb in hbuf]
    U = [k.sb([128, Q], name=f"U{c}") for c in range(2)]
    gre_ = [k.sb([128, Q], name=f"gre{i}") for i in range(2)]; gim_ = [k.sb([128, Q], name=f"gim{i}") for i in range(2)]
    Gre_ = [k.sb([128, Q], name=f"Gre{i}") for i in range(2)]; Gim_ = [k.sb([128, Q], name=f"Gim{i}") for i in range(2)]
    hre = [k.sb([128, Q], name=f"hre{i}") for i in range(2)]; nhim = [k.sb([128, Q], name=f"nhim{i}") for i in range(2)]
    t1_ = [k.sb([128, Q], name=f"t1_{i}") for i in range(2)]; t2_ = [k.sb([128, Q], name=f"t2_{i}") for i in range(2)]
    car = k.sb([128, 12, 2], name="car"); ct_ = [k.sb([128, 2], name=f"ct{i}") for i in range(2)]
    V("dve", lambda e: e.memset(car[:, :, :], 0.0), [], [car])
    og = [k.sb([128, 2, Q], name=f"og{i}") for i in range(2)]
    hi = 0
    for d in range(2):
        order = list(range(NCH)) if d == 0 else [0] + list(range(NCH - 1, 0, -1))
        rv = (lambda ap: ap[:, ::-1]) if d == 1 else (lambda ap: ap)
        for ch in order:
            hb = hbuf[hi % 2]
            hviews = hvs[hi % 2]
            hi += 1
            load_h(k, hall, hviews, hb, ch)

            def ev_u(mc, ps):
                V("act", lambda e, mc=mc, ps=ps: e.activation(out=R(U[mc][:, :]), in_=ps[:, 0:Q], func=AF.Copy), [ps], [U[mc]])
            linear(k, w_u, range(2), 8, hviews, Q, ev_u)
            def tile_body(i, V):
                gre, gim, Gre, Gim = gre_[i % 2], gim_[i % 2], Gre_[i % 2], Gim_[i % 2]
                t1, t2, ct = t1_[i % 2], t2_[i % 2], ct_[i % 2]
                di = d * 6 + i
                c = 0 if i < 4 else 1
                pb_re, pb_im = psb[(i % 2) * 2], psb[(i % 2) * 2 + 1]
                V("pe", lambda e, di=di, c=c, p=pb_re: e.matmul(p[:, 0:Q], breT[:, di, :], R(U[c][:, :]), start=True, stop=True),
                  [breT, U[c]], [pb_re])
                V("pe", lambda e, di=di, c=c, p=pb_im: e.matmul(p[:, 0:Q], bimT[:, di, :], R(U[c][:, :]), start=True, stop=True),
                  [bimT, U[c]], [pb_im])
                cs, sn = cosT[:, di, :], sinT[:, di, :]
                V("dve", lambda e, p=pb_re, cs=cs, rv=rv: e.tensor_tensor(out=t1[:, :], in0=rv(p[:, 0:Q]), in1=cs, op=ALU.mult), [pb_re, cosT], [t1])
                V("dve", lambda e, p=pb_im, sn=sn, rv=rv: e.tensor_tensor(out=t2[:, :], in0=rv(p[:, 0:Q]), in1=sn, op=ALU.mult), [pb_im, sinT], [t2])
                V("dve", lambda e: e.tensor_tensor(out=gre[:, :], in0=t1[:, :], in1=t2[:, :], op=ALU.add), [t1, t2], [gre])
                V("dve", lambda e, p=pb_im, cs=cs, rv=rv: e.tensor_tensor(out=t1[:, :], in0=rv(p[:, 0:Q]), in1=cs, op=ALU.mult), [pb_im, cosT], [t1])
                V("dve", lambda e, p=pb_re, sn=sn, rv=rv: e.tensor_tensor(out=t2[:, :], in0=rv(p[:, 0:Q]), in1=sn, op=ALU.mult), [pb_re, sinT], [t2])
                V("dve", lambda e: e.tensor_tensor(out=gim[:, :], in0=t1[:, :], in1=t2[:, :], op=ALU.subtract), [t1, t2], [gim])
                V("dve", lambda e, di=di: e.tensor_tensor_scan(out=Gre[:, :], data0=rr[:, di:di + 1].to_broadcast([128, Q]), data1=gre[:, :],
                                                               initial=car[:, di, 0:1], op0=ALU.mult, op1=ALU.add), [rr, gre, car], [Gre])
                V("dve", lambda e, di=di: e.tensor_tensor_scan(out=Gim[:, :], data0=rr[:, di:di + 1].to_broadcast([128, Q]), data1=gim[:, :],
                                                               initial=car[:, di, 1:2], op0=ALU.mult, op1=ALU.add), [rr, gim, car], [Gim])
                hr, nh = hre[i % 2], nhim[i % 2]
                V("dve", lambda e, cs=cs: e.tensor_tensor(out=t1[:, :], in0=Gre[:, :], in1=cs, op=ALU.mult), [Gre, cosT], [t1])
                V("dve", lambda e, sn=sn: e.tensor_tensor(out=t2[:, :], in0=Gim[:, :], in1=sn, op=ALU.mult), [Gim, sinT], [t2])
                V("dve", lambda e, hr=hr: e.tensor_tensor(out=R(hr[:, :]), in0=t1[:, :], in1=t2[:, :], op=ALU.subtract), [t1, t2], [hr])
                V("dve", lambda e, sn=sn: e.tensor_tensor(out=t1[:, :], in0=Gre[:, :], in1=sn, op=ALU.mult), [Gre, sinT], [t1])
                V("dve", lambda e, cs=cs: e.tensor_tensor(out=t2[:, :], in0=Gim[:, :], in1=cs, op=ALU.mult), [Gim, cosT], [t2])
                V("dve", lambda e, nh=nh: e.scalar_tensor_tensor(out=R(nh[:, :]), in0=t1[:, :], scalar=-1.0, in1=t2[:, :],
                                                                 op0=ALU.mult, op1=ALU.subtract), [t1, t2], [nh])
                hl = hr[:, Q - 1:Q].bitcast(F32) if False else hr[:, Q - 1:Q]
                nl = nh[:, Q - 1:Q]
                V("dve", lambda e, di=di, nl=nl: e.tensor_scalar(out=ct[:, 0:1], in0=nl, scalar1=sinT[:, di, 1:2], scalar2=None,
                                                                 op0=ALU.mult), [nh, sinT], [ct])
                V("dve", lambda e, di=di, hl=hl: e.scalar_tensor_tensor(out=car[:, di, 0:1], in0=hl, scalar=cosT[:, di, 1:2],
                                                                        in1=ct[:, 0:1], op0=ALU.mult, op1=ALU.add), [hr, cosT, ct], [car])
                V("dve", lambda e, di=di, nl=nl: e.tensor_scalar(out=ct[:, 1:2], in0=nl, scalar1=cosT[:, di, 1:2], scalar2=None,
                                                                 op0=ALU.mult), [nh, cosT], [ct])
                V("dve", lambda e, di=di, hl=hl: e.scalar_tensor_tensor(out=car[:, di, 1:2], in0=hl, scalar=sinT[:, di, 1:2],
                                                                        in1=ct[:, 1:2], op0=ALU.mult, op1=ALU.subtract), [hr, sinT, ct], [car])
                first = (i == 0) or (i == 4)
                last = (i == 3) or (i == 5)
                V("pe", lambda e, di=di, c=c, hr=hr, first=first: e.matmul(psy[c][:, 0:Q], cre[:, di, :], R(hr[:, :]), start=first, stop=False),
                  [cre, hr], [psy[c]])
                V("pe", lambda e, di=di, c=c, nh=nh, last=last: e.matmul(psy[c][:, 0:Q], cim[:, di, :], R(nh[:, :]), start=False, stop=last),
                  [cim, nh], [psy[c]])
            for ia in (0, 2, 4):
                lists = []
                for i in (ia, ia + 1):
                    cur = []
                    tile_body(i, lambda eng, fn, r, w, cur=cur: cur.append((eng, fn, r, w)))
                    lists.append(cur)
                for j in range(max(len(lists[0]), len(lists[1]))):
                    for cur in lists:
                        if j < len(cur):
                            V(*cur[j])
            for c in range(2):
                ya = YAv[c][ch]
                if d == 0:
                    V("dve", lambda e, c=c, ya=ya: e.scalar_tensor_tensor(out=ya[:, :], in0=U[c][:, :], scalar=dsk[:, c:c + 1],
                                                                          in1=psy[c][:, 0:Q], op0=ALU.mult, op1=ALU.add), [U[c], dsk, psy[c]], [ya])
                else:
                    o = og[(hi) % 2]
                    V("dve", lambda e, c=c, ya=ya: e.tensor_tensor(out=ya[:, :], in0=ya[:, :], in1=psy[c][:, 0:Q][:, ::-1], op=ALU.add),
                      [ya, psy[c]], [ya])
                    V("act", lambda e, c=c, ya=ya, o=o: e.activation(out=o[:, c, :], in_=ya[:, :], func=AF.Gelu), [ya], [o])
            if d == 1:
                k.dma(ymix, ydst(ymix, 0, ch), o, o[:], q="act")
    k.phase_end()


def phase_SSD(k, P, hall, ymix):
    k.phase_begin("SSD")
    w_xbc, w_dt, conv_w, conv_b = P["w_xbc"], P["w_dt"], P["sconv_w"], P["sconv_b"]
    alog, dtb, dskip, masks, ident = P["alog"], P["dtb"], P["sdskip"], P["masks"], P["ident"]
    V = lambda eng, fn, r, w: k.op(eng, fn, reads=r, writes=w)

    k.wbuf = [k.sb([128, 8, 128], F32R, name=f"wb{i}") for i in range(2)]
    k.wstg = [k.sb([128, 8, 128], name=f"wstg{i}") for i in range(4)]
    k.si = 0
    k.wi = 0
    k.psl = [k.ps([128, 512], name=f"psu{i}") for i in range(2)]
    k.pi = 0
    pst = k.ps([128, 512], name="pst")
    ptr = [k.psview(pst, pst.ap[:, i * 128:(i + 1) * 128], f"ptr{i}") for i in range(3)]
    pdt = k.psview(pst, pst.ap[:, 384:388], "pdt"); pcol = k.psview(pst, pst.ap[:, 392:396], "pcol")
    prow = k.ps([128, 512], name="prow")
    pss = k.ps([128, 512], name="pss")
    psc = k.psview(pss, pss.ap[:, 0:128], "psc"); pstate = k.psview(pss, pss.ap[:, 256:512], "pstate")
    pys = [k.ps([128, 512], name=f"py{i}") for i in range(2)]

    def ld(name, src, shape, q="sp", dt=F32):
        t = k.sb(list(shape), dt, name=name)
        k.dma(t, t[:], src, src[:], q=q)
        return t
    cw = ld("cw", conv_w, [128, 4, 4]); cb = ld("cb", conv_b, [128, 4])
    al = ld("al", alog, [128, 2, 4]); db = ld("db", dtb, [128, 2, 4]); dsk = ld("dsk", dskip, [128, 2])
    mk32 = ld("mk32", masks, [128, 2, 128]); mkr = ld("mkr", masks, [128, 2, 128], q="pool", dt=F32R)
    idt = ld("idt", ident, [128, 128]); idr = ld("idr", ident, [128, 128], q="pool", dt=F32R)
    wdt = ld("wdt", w_dt, [128, 8, 4], q="pool", dt=F32R)
    na = k.sb([128, 2, 4], name="na")
    V("act", lambda e: e.activation(out=na[:, :, :], in_=al[:, :, :], func=AF.Exp), [al], [na])
    V("dve", lambda e: e.tensor_single_scalar(out=na[:, :, :], in_=na[:, :, :], scalar=-1.0, op=ALU.mult), [na], [na])
    ones32 = k.sb([128, 128], name="ones32"); ones = k.sb([128, 128], F32R, name="ones")
    V("dve", lambda e: e.memset(ones32[:, :], 1.0), [], [ones32])
    V("act", lambda e: e.activation(out=ones[:, :], in_=ones32[:, :], func=AF.Copy), [ones32], [ones])

    YACC = k.sb([128, 2, NCH * Q], name="YACC")
    YV = [[Buf(YACC.ap[:, c, j * 128:(j + 1) * 128], f"Y{c}_{j}") for j in range(2 * NCH)] for c in range(2)]
    hbt = [k.sb([128, 8, HW_], name=f"hb{i}") for i in range(2)]
    hvs = [t.views(8) for t in hbt]
    XCt = k.sb([128, 4, Q], name="XC"); XC = XCt.views(4)
    cv = k.sb([128, Q], name="cv")
    dt_ = k.sb([128, 4], name="dt"); adt = k.sb([128, 4], name="adt"); acol = k.sb([128, 4], name="acol")
    dte = k.sb([128, 4], name="dte"); cd = k.sb([128, 4], name="cd")
    arhs = k.sb([128, 512], name="arhs"); eac = k.sb([128, 512], name="eac")
    M1 = k.sb([128, 128], name="M1"); Lt = [k.sb([128, 128], name=f"Lt{i}") for i in range(2)]
    Mt = [k.sb([128, 128], name=f"Mt{i}") for i in range(4)]; CE = [k.sb([128, 128], name=f"CE{i}") for i in range(4)]
    XDT = [k.sb([128, 128], name=f"XDT{i}") for i in range(4)]; XDTE = k.sb([128, 256], name="XDTE")
    Btok = k.sb([128, 128], name="Btok")
    HS32 = [k.sb([128, 64], name=f"HS32_{i}") for i in range(4)]; HSr = [k.sb([128, 128], name=f"HSr{i}") for i in range(4)]
    z128 = k.sb([128, 128], name="z128")
    V("dve", lambda e: e.memset(z128[:, :], 0.0), [], [z128])
    for h in range(4):
        V("act", lambda e, h=h: e.activation(out=R(XDT[h][:, :]), in_=z128[:, :], func=AF.Copy), [z128], [XDT[h]])
    og = [k.sb([128, 2, Q], name=f"og{i}") for i in range(2)]
    zt = k.sb([128, 8, 2], name="zt")
    V("dve", lambda e: e.memset(zt[:, :, :], 0.0), [], [zt])
    hi = 0
    for d in range(2):
        for h in range(4):
            V("act", lambda e, h=h: e.activation(out=R(HSr[h][:, :]), in_=z128[:, :], func=AF.Copy), [z128], [HSr[h]])
            V("dve", lambda e, h=h: e.memset(HS32[h][:, :], 0.0), [], [HS32[h]])
        order = list(range(NCH)) if d == 0 else [0] + list(range(NCH - 1, 0, -1))
        for ch in order:
            hv = hvs[hi % 2]; hb = hbt[hi % 2]; o = og[hi % 2]; hi += 1
            load_h(k, hall, hv, hb, ch, off=2, zt=zt)

            def ev_c(mc, ps):
                V("dve", lambda e, mc=mc, ps=ps: e.tensor_scalar(out=cv[:, :], in0=ps[:, 0:Q], scalar1=cw[:, mc, 0:1], scalar2=cb[:, mc:mc + 1],
                                                                 op0=ALU.mult, op1=ALU.add), [ps, cw, cb], [cv])
                for kk in range(1, 4):
                    V("dve", lambda e, mc=mc, ps=ps, kk=kk: e.scalar_tensor_tensor(out=cv[:, :], in0=ps[:, kk:kk + Q], scalar=cw[:, mc, kk:kk + 1],
                                                                                   in1=cv[:, :], op0=ALU.mult, op1=ALU.add), [ps, cw, cv], [cv])
                V("act", lambda e, mc=mc: e.activation(out=R(XC[mc][:, :]), in_=cv[:, :], func=AF.Silu), [cv], [XC[mc]])
            linear(k, w_xbc, range(4), 8, hv, HW_, ev_c)
            for sub in ((0, 1) if d == 0 else (1, 0)):
                j = ch * 2 + sub
                s0 = sub * 128
                V("pe", lambda e, s0=s0: e.matmul(psc[:, :], R(XC[2][:, s0:s0 + 128]), R(XC[3][:, s0:s0 + 128]), start=True, stop=True),
                  [XC[2], XC[3]], [psc])
                V("dve", lambda e, d=d: e.tensor_tensor(out=M1[:, :], in0=psc[:, :], in1=mk32[:, d, :], op=ALU.mult), [psc, mk32], [M1])
                for t_i, src in enumerate((XC[0], XC[1], XC[2])):
                    V("pe", lambda e, t_i=t_i, src=src, s0=s0: e.transpose(out=ptr[t_i][:, :], in_=src[:, s0:s0 + 128], identity=idt[:, :]),
                      [src, idt], [ptr[t_i]])
                V("act", lambda e: e.activation(out=R(Btok[:, :]), in_=ptr[2][:, :], func=AF.Copy), [ptr[2]], [Btok])
                for kc in range(8):
                    V("pe", lambda e, kc=kc, hv=hv, s0=s0: e.matmul(pdt[:, :], R(hv[kc][:, 2 + s0:2 + s0 + 128]), wdt[:, kc, :],
                                                                    start=(kc == 0), stop=(kc == 7)), [hv[kc], wdt], [pdt])
                V("dve", lambda e, d=d: e.tensor_tensor(out=dt_[:, :], in0=pdt[:, :], in1=db[:, d, :], op=ALU.add), [pdt, db], [dt_])
                V("act", lambda e: e.activation(out=dt_[:, :], in_=dt_[:, :], func=AF.Exp), [dt_], [dt_])
                V("act", lambda e: e.activation(out=dt_[:, :], in_=dt_[:, :], func=AF.Ln, bias=1.0, scale=1.0), [dt_], [dt_])
                V("dve", lambda e, d=d: e.tensor_tensor(out=R(adt[:, :]), in0=dt_[:, :], in1=na[:, d, :], op=ALU.mult), [dt_, na], [adt])
                V("pe", lambda e, d=d: e.matmul(pcol[:, :], mkr[:, d, :], R(adt[:, :]), start=True, stop=True), [mkr, adt], [pcol])
                V("act", lambda e: e.activation(out=acol[:, :], in_=pcol[:, :], func=AF.Copy), [pcol], [acol])
                for h in range(4):
                    V("dve", lambda e, h=h, d=d: e.tensor_scalar(out=R(arhs[:, h * 128:(h + 1) * 128]), in0=mk32[:, d, :], scalar1=adt[:, h:h + 1], scalar2=None,
                                                                 op0=ALU.mult), [mk32, adt], [arhs])
                V("pe", lambda e: e.matmul(prow[:, :], ones[:, :], R(arhs[:, :]), start=True, stop=True), [ones, arhs], [prow])
                V("act", lambda e: e.activation(out=eac[:, :], in_=prow[:, :], func=AF.Exp), [prow], [eac])
                tcol = 127 if d == 0 else 0
                totv = prow[:, :].rearrange("p (h l) -> p h l", h=4)[:, :, tcol]
                V("dve", lambda e, totv=totv: e.tensor_tensor(out=dte[:, :], in0=totv, in1=acol[:, :], op=ALU.subtract), [prow, acol], [dte])
                V("act", lambda e: e.activation(out=dte[:, :], in_=dte[:, :], func=AF.Exp), [dte], [dte])
                V("act", lambda e, totv=totv: e.activation(out=cd[:, :], in_=totv, func=AF.Exp), [prow], [cd])
                for h in range(4):
                    c, hb_ = h // 2, (h % 2) * 64
                    V("dve", lambda e, h=h, c=c, hb_=hb_: e.tensor_scalar(out=R(XDT[h][:, hb_:hb_ + 64]), in0=ptr[c][:, hb_:hb_ + 64],
                                                                          scalar1=dt_[:, h:h + 1], scalar2=None, op0=ALU.mult), [ptr[c], dt_], [XDT[h]])
                    V("dve", lambda e, h=h, c=c, hb_=hb_: e.tensor_scalar(out=R(XDTE[:, h * 64:h * 64 + 64]), in0=ptr[c][:, hb_:hb_ + 64],
                                                                          scalar1=dt_[:, h:h + 1], scalar2=dte[:, h:h + 1], op0=ALU.mult, op1=ALU.mult),
                      [ptr[c], dt_, dte], [XDTE])
                    lt = Lt[h % 2]
                    V("dve", lambda e, h=h, lt=lt: e.tensor_scalar(out=lt[:, :], in0=prow[:, h * 128:(h + 1) * 128], scalar1=acol[:, h:h + 1], scalar2=0.0,
                                                                   op0=ALU.subtract, op1=ALU.min), [prow, acol], [lt])
                    V("act", lambda e, lt=lt: e.activation(out=lt[:, :], in_=lt[:, :], func=AF.Exp), [lt], [lt])
                    V("dve", lambda e, h=h, lt=lt: e.tensor_tensor(out=R(Mt[h][:, :]), in0=lt[:, :], in1=M1[:, :], op=ALU.mult), [lt, M1], [Mt[h]])
                    V("pool", lambda e, h=h, s0=s0: e.tensor_tensor(out=R(CE[h][:, :]), in0=XC[3][:, s0:s0 + 128], in1=eac[:, h * 128:(h + 1) * 128], op=ALU.mult),
                      [XC[3], eac], [CE[h]])
                for c in range(2):
                    for hh in range(2):
                        h = c * 2 + hh
                        V("pe", lambda e, c=c, h=h, hh=hh: e.matmul(pys[c][:, 0:128], R(XDT[h][:, :]), R(Mt[h][:, :]), start=(hh == 0), stop=False),
                          [XDT[h], Mt[h]], [pys[c]])
                        V("pe", lambda e, c=c, h=h, hh=hh: e.matmul(pys[c][:, 0:128], R(HSr[h][:, :]), R(CE[h][:, :]), start=False, stop=(hh == 1)),
                          [HSr[h], CE[h]], [pys[c]])
                    yv = YV[c][j]
                    if d == 0:
                        V("dve", lambda e, c=c, yv=yv, s0=s0: e.scalar_tensor_tensor(out=yv[:, :], in0=XC[c][:, s0:s0 + 128], scalar=dsk[:, c:c + 1],
                                                                                     in1=pys[c][:, 0:128], op0=ALU.mult, op1=ALU.add), [XC[c], dsk, pys[c]], [yv])
                    else:
                        V("dve", lambda e, c=c, yv=yv, s0=s0, o=o: e.tensor_tensor(out=o[:, c, s0:s0 + 128], in0=yv[:, :], in1=pys[c][:, 0:128], op=ALU.add),
                          [yv, pys[c]], [o])
                V("pe", lambda e: e.matmul(pstate[:, :], R(Btok[:, :]), R(XDTE[:, :]), start=True, stop=True), [Btok, XDTE], [pstate])
                for h in range(4):
                    hb_ = (h % 2) * 64
                    V("dve", lambda e, h=h: e.scalar_tensor_tensor(out=HS32[h][:, :], in0=HS32[h][:, :], scalar=cd[:, h:h + 1],
                                                                   in1=pstate[:, h * 64:(h + 1) * 64], op0=ALU.mult, op1=ALU.add), [HS32[h], cd, pstate], [HS32[h]])
                    V("act", lambda e, h=h, hb_=hb_: e.activation(out=R(HSr[h][:, hb_:hb_ + 64]), in_=HS32[h][:, :], func=AF.Copy), [HS32[h]], [HSr[h]])
            if d == 1:
                k.dma(ymix, ydst(ymix, 2, ch), o, o[:], q="act")
    k.phase_end()


OFF = [2] + [261 + (ch - 1) * Q for ch in range(1, NCH)]
XW = 261 + 32 * Q + 3


def phase_LRU(k, P, hall, ymix):
    k.phase_begin("LRU")
    w_x, w_gt, conv_w, conv_b, WG, BG, lam = P["w_x"], P["w_gt"], P["lconv_w"], P["lconv_b"], P["WG"], P["BG"], P["llam"]
    V = lambda eng, fn, r, w: k.op(eng, fn, reads=r, writes=w)

    k.wbuf = [k.sb([128, 8, 128], F32R, name=f"wb{i}") for i in range(2)]
    k.wstg = [k.sb([128, 8, 128], name=f"wstg{i}") for i in range(4)]
    k.si = 0
    k.wi = 0
    k.psl = [k.ps([128, 512], name=f"psu{i}") for i in range(4)]
    k.pi = 0
    psg = [k.ps([128, 512], name=f"psg{i}") for i in range(4)]

    def ld(name, src, shape, q="sp", dt=F32):
        t = k.sb(list(shape), dt, name=name)
        k.dma(t, t[:], src, src[:], q=q)
        return t
    cw = ld("cw", conv_w, [128, 2, 4]); cb = ld("cb", conv_b, [128, 2])
    wg = ld("wg", WG, [128, 8, 128], q="pool", dt=F32R); bg = ld("bg", BG, [128, 8]); lm = ld("lm", lam, [128, 4])
    ls8 = k.sb([128, 4], name="ls8"); ls16 = k.sb([128, 4], name="ls16")
    V("act", lambda e: e.activation(out=ls8[:, :], in_=lm[:, :], func=AF.Exp, scale=-1.0), [lm], [ls8])
    V("act", lambda e: e.activation(out=ls8[:, :], in_=ls8[:, :], func=AF.Ln, bias=1.0, scale=1.0), [ls8], [ls8])
    V("dve", lambda e: e.tensor_single_scalar(out=ls16[:, :], in_=ls8[:, :], scalar=-16.0, op=ALU.mult), [ls8], [ls16])
    V("dve", lambda e: e.tensor_single_scalar(out=ls8[:, :], in_=ls8[:, :], scalar=-8.0, op=ALU.mult), [ls8], [ls8])

    XA = k.sb([128, 2, XW + 128], name="XA")
    V("pool", lambda e: e.memset(XA[:, :, :], 0.0), [], [XA])
    HF = k.sb([128, 2, NCH * Q + 128], name="HF")
    HFv = [[Buf(HF.ap[:, c, ch * Q:(ch + 1) * Q], f"HF{c}_{ch}") for ch in range(NCH)] for c in range(2)]
    hbt = [k.sb([128, 8, Q], name=f"hb{i}") for i in range(2)]
    hvs = [t.views(8) for t in hbt]
    hi = 0
    for ch in range(NCH):
        hv = hvs[hi % 2]; hb = hbt[hi % 2]; hi += 1
        load_h(k, hall, hv, hb, ch)

        def ev_x(mc, ps, ch=ch):
            if ch == 0:
                V("act", lambda e, mc=mc, ps=ps: e.activation(out=XA[:, mc, 2:2 + Q], in_=ps[:, 0:Q], func=AF.Copy), [ps], [XA])
            else:
                base = 261 + 4 * (ch - 1)
                ov = XA.ap[:, mc, base:base + 8192].rearrange("p (j i) -> p i j", i=128)[:, 0:4, :]
                V("act", lambda e, ov=ov, ps=ps: e.activation(out=ov, in_=ps[:, 0:Q].rearrange("p (i j) -> p i j", j=64), func=AF.Copy),
                  [ps], [XA])
        linear(k, w_x, range(2), 8, hv, Q, ev_x)

    xc = [k.sb([128, Q], name=f"xc{i}") for i in range(2)]
    xcr = [k.sb([128, Q], name=f"xcr{i}") for i in range(2)]
    ra = [k.sb([128, Q], name=f"ra{i}") for i in range(2)]; ix = [k.sb([128, Q], name=f"ix{i}") for i in range(2)]
    At = [k.sb([128, Q], name=f"A{i}") for i in range(2)]; sq = [k.sb([128, Q], name=f"sq{i}") for i in range(2)]
    HB = [[k.sb([128, Q], name=f"HB{c}_{i}") for i in range(2)] for c in range(2)]
    gg = [k.sb([128, Q], name=f"gg{i}") for i in range(2)]
    og = [k.sb([128, 2, Q], name=f"og{i}") for i in range(2)]
    step = 0
    for d in range(2):
        order = list(range(NCH)) if d == 0 else [0] + list(range(NCH - 1, 0, -1))
        prev = None
        for oi, ch in enumerate(order):
            step += 1
            def tile_body(c, V):
                x_ = xc[c]
                o0 = OFF[ch]
                V("dve", lambda e, c=c, x_=x_, o0=o0: e.tensor_scalar(out=x_[:, :], in0=XA[:, c, o0 - 2:o0 - 2 + Q], scalar1=cw[:, c, 0:1],
                                                                     scalar2=cb[:, c:c + 1], op0=ALU.mult, op1=ALU.add), [XA, cw, cb], [x_])
                for kk in range(1, 4):
                    ov = R(xcr[c][:, :]) if kk == 3 else x_[:, :]
                    ob = xcr[c] if kk == 3 else x_
                    V("dve", lambda e, c=c, x_=x_, o0=o0, kk=kk, ov=ov: e.scalar_tensor_tensor(
                        out=ov, in0=XA[:, c, o0 - 2 + kk:o0 - 2 + kk + Q], scalar=cw[:, c, kk:kk + 1], in1=x_[:, :],
                        op0=ALU.mult, op1=ALU.add), [XA, cw, x_], [ob])
                x_ = xcr[c]
                pa, px = psg[c * 2], psg[c * 2 + 1]
                ia, ixx = (d * 2 + 0) * 2 + c, (d * 2 + 1) * 2 + c
                V("pe", lambda e, pa=pa, ia=ia, x_=x_: e.matmul(pa[:, 0:Q], wg[:, ia, :], R(x_[:, :]), start=True, stop=True), [wg, x_], [pa])
                V("pe", lambda e, px=px, ixx=ixx, x_=x_: e.matmul(px[:, 0:Q], wg[:, ixx, :], R(x_[:, :]), start=True, stop=True), [wg, x_], [px])
                V("act", lambda e, c=c, pa=pa, ia=ia: e.activation(out=ra[c][:, :], in_=pa[:, 0:Q], func=AF.Sigmoid, bias=bg[:, ia:ia + 1], scale=1.0),
                  [pa, bg], [ra[c]])
                V("act", lambda e, c=c, px=px, ixx=ixx: e.activation(out=ix[c][:, :], in_=px[:, 0:Q], func=AF.Sigmoid, bias=bg[:, ixx:ixx + 1], scale=1.0),
                  [px, bg], [ix[c]])
                li = d * 2 + c
                V("act", lambda e, c=c, li=li: e.activation(out=At[c][:, :], in_=ra[c][:, :], func=AF.Exp, scale=ls8[:, li:li + 1]), [ra[c], ls8], [At[c]])
                V("act", lambda e, c=c, li=li: e.activation(out=sq[c][:, :], in_=ra[c][:, :], func=AF.Exp, scale=ls16[:, li:li + 1]), [ra[c], ls16], [sq[c]])
                V("act", lambda e, c=c: e.activation(out=sq[c][:, :], in_=sq[c][:, :], func=AF.Sqrt, scale=-1.0, bias=1.0), [sq[c]], [sq[c]])
                V("dve", lambda e, c=c: e.tensor_tensor(out=sq[c][:, :], in0=sq[c][:, :], in1=ix[c][:, :], op=ALU.mult), [sq[c], ix[c]], [sq[c]])
                V("dve", lambda e, c=c, x_=x_: e.tensor_tensor(out=sq[c][:, :], in0=sq[c][:, :], in1=x_[:, :], op=ALU.mult), [sq[c], x_], [sq[c]])
                if d == 0:
                    dst = HFv[c][ch]
                    if prev is None:
                        ini, inib = 0.0, []
                    else:
                        ini, inib = HFv[c][prev][:, Q - 1:Q], [HFv[c][prev]]
                    V("dve", lambda e, c=c, dst=dst, ini=ini: e.tensor_tensor_scan(out=dst[:, :], data0=At[c][:, :], data1=sq[c][:, :], initial=ini,
                                                                                  op0=ALU.mult, op1=ALU.add), [At[c], sq[c]] + inib, [dst])
                else:
                    dst = HB[c][oi % 2]
                    if prev is None:
                        ini, inib = 0.0, []
                    else:
                        pb = HB[c][(oi - 1) % 2]
                        ini, inib = pb[:, 0:1], [pb]
                    V("dve", lambda e, c=c, dst=dst, ini=ini: e.tensor_tensor_scan(out=dst[:, ::-1], data0=At[c][:, ::-1], data1=sq[c][:, ::-1],
                                                                                  initial=ini, op0=ALU.mult, op1=ALU.add), [At[c], sq[c]] + inib, [dst])
            lists = []
            for c in range(2):
                cur = []
                tile_body(c, lambda eng, fn, r, w, cur=cur: cur.append((eng, fn, r, w)))
                lists.append(cur)
            for j in range(max(len(lists[0]), len(lists[1]))):
                for cur in lists:
                    if j < len(cur):
                        V(*cur[j])
            if d == 1:
                for c in range(2):
                    dst = HB[c][oi % 2]
                    V("pool", lambda e, c=c, dst=dst, ch=ch: e.tensor_tensor(out=HFv[c][ch][:, :], in0=HFv[c][ch][:, :], in1=dst[:, :], op=ALU.add),
                      [HFv[c][ch], dst], [HFv[c][ch]])
            prev = ch
    for ch in range(NCH):
        hv = hvs[hi % 2]; hb = hbt[hi % 2]; o = og[hi % 2]; hi += 1
        load_h(k, hall, hv, hb, ch)

        def ev_g(mc, ps, ch=ch, o=o):
            V("act", lambda e, mc=mc, ps=ps: e.activation(out=gg[mc][:, :], in_=ps[:, 0:Q], func=AF.Gelu), [ps], [gg[mc]])
            if ch == 0:
                V("dve", lambda e, mc=mc, o=o: e.tensor_tensor(out=o[:, mc, :], in0=HF[:, mc, 0:Q], in1=gg[mc][:, :], op=ALU.mult),
                  HFv[mc] + [gg[mc]], [o])
            else:
                base = 256 + 4 * (ch - 1)
                hvw = HF.ap[:, mc, base:base + 8192].rearrange("p (j i) -> p i j", i=128)[:, 0:4, :]
                V("dve", lambda e, mc=mc, o=o, hvw=hvw: e.tensor_tensor(out=o[:, mc, :].rearrange("p (i j) -> p i j", j=64), in0=hvw,
                                                                        in1=gg[mc][:, :].rearrange("p (i j) -> p i j", j=64), op=ALU.mult),
                  HFv[mc] + [gg[mc]], [o])
        linear(k, w_gt, range(2), 8, hv, Q, ev_g)
        k.dma(ymix, ydst(ymix, 1, ch), o, o[:], q="act")
    k.phase_end()


def wrel(w, pad_k=None):
    K, M = w.shape
    Kp = -(-K // 128) * 128; Mp = -(-M // 128) * 128
    if (Kp, Mp) != (K, M):
        wp = np.zeros((Kp, Mp), w.dtype); wp[:K, :M] = w; w = wp
    return np.ascontiguousarray(w.reshape(Kp // 128, 128, Mp // 128, 128).transpose(2, 1, 0, 3))

def vrel(v):
    return np.ascontiguousarray(v.reshape(-1, 128).T)

def fm_blocks(lat, ctx, NB=4):
    T, F = lat.shape
    tb = T // NB
    out = []
    for b in range(NB):
        t = lat[b * tb:(b + 1) * tb]
        if ctx is not None:
            cb = ctx.shape[0] // NB
            t = np.concatenate([t, ctx[b * cb:(b + 1) * cb]], 0)
        out.append(t.T.reshape(F // 128, 128, -1).transpose(1, 0, 2))
    return np.ascontiguousarray(np.stack(out))

def un_blocks(a, has_ctx, NB=4):
    NB, P, C, W = a.shape
    t = a.transpose(0, 3, 2, 1).reshape(NB, W, C * P)
    lat = t[:, :512].reshape(NB * 512, C * P)
    ctx = t[:, 512:].reshape(-1, C * P) if has_ctx else None
    return lat, ctx

def seq_chunks(hfull, Q=256):
    T, F = hfull.shape
    return np.ascontiguousarray(hfull.reshape(T // Q, Q, F // 128, 128).transpose(0, 3, 2, 1))

def s5_params(inp, l, q):
    lamre = np.zeros((128, 12), np.float32); lamim = np.zeros((128, 12), np.float32); logdt = np.zeros((128, 12), np.float32)
    Bre = np.zeros((128, 12, 128), np.float32); Bim = np.zeros_like(Bre); CreT = np.zeros_like(Bre); CimT = np.zeros_like(Bre)
    for d in range(2):
        for i in range(6):
            di = d * 6 + i
            for gg in range(2):
                g = 12 * q + 2 * i + gg
                ps = slice(gg * 64, gg * 64 + 64)
                lamre[ps, di] = inp["s5_lam_re"][l, d, g]; lamim[ps, di] = inp["s5_lam_im"][l, d, g]
                logdt[ps, di] = inp["s5_log_dt"][l, d, g]
                c0 = 32 * (i % 4) + gg * 16
                Bre[ps, di, c0:c0 + 16] = inp["s5_b_re"][l, d, g]; Bim[ps, di, c0:c0 + 16] = inp["s5_b_im"][l, d, g]
                CreT[ps, di, c0:c0 + 16] = inp["s5_c_re"][l, d, g].T; CimT[ps, di, c0:c0 + 16] = inp["s5_c_im"][l, d, g].T
    dsk = np.zeros((128, 2), np.float32)
    dv = inp["s5_d"][l, 192 * q:192 * q + 192]
    dsk[:, 0] = dv[:128]; dsk[:64, 1] = dv[128:]
    return dict(lamre=lamre, lamim=lamim, logdt=logdt, Bre=Bre, Bim=Bim, CreT=CreT, CimT=CimT, dskip=dsk,
                w_u=wrel(inp["w_in"][l][:, 192 * q:192 * q + 192]))

def to_cm(lat):
    T, F = lat.shape
    return lat.reshape(128, 64, F).transpose(1, 0, 2).reshape(T, F)

def from_cm(o):
    T, F = o.shape
    return o.reshape(64, 128, F).transpose(1, 0, 2).reshape(T, F)

def lru_params(inp, l, q):
    WG = np.zeros((128, 8, 128), np.float32); BG = np.zeros((128, 8), np.float32); lam = np.zeros((128, 4), np.float32)
    for d in range(2):
        for kind, (wn, bn) in enumerate((("lru_w_a", "lru_b_a"), ("lru_w_x", "lru_b_x"))):
            for c in range(2):
                idx = (d * 2 + kind) * 2 + c
                for bb in range(2):
                    blk = 4 * q + 2 * c + bb
                    WG[bb * 64:(bb + 1) * 64, idx, bb * 64:(bb + 1) * 64] = inp[wn][l, d, blk]
                BG[:, idx] = inp[bn][l, d, 256 * q + 128 * c:256 * q + 128 * c + 128]
        for c in range(2):
            lam[:, d * 2 + c] = inp["lru_lam"][l, d, 256 * q + 128 * c:256 * q + 128 * c + 128]
    ch = slice(256 * q, 256 * q + 256)
    cw = np.ascontiguousarray(inp["lru_conv_w"][l][:, ch].T.reshape(2, 128, 4).transpose(1, 0, 2))
    cb = np.ascontiguousarray(inp["lru_conv_b"][l][ch].reshape(2, 128).T)
    return dict(WG=WG, BG=BG, llam=lam, lconv_w=cw, lconv_b=cb,
                w_x=wrel(inp["w_in"][l][:, 768 + 256 * q:768 + 256 * q + 256]),
                w_gt=wrel(inp["w_in"][l][:, 1792 + 256 * q:1792 + 256 * q + 256]))

def halo_chunks(hctx, hlat, Q=256):
    F = hctx.shape[1]
    out = np.zeros((33, Q + 4, F), np.float32)
    out[0, 2:2 + Q] = hctx
    lp = np.zeros((hlat.shape[0] + 4, F), np.float32); lp[2:-2] = hlat
    for ch in range(32):
        out[ch + 1] = lp[ch * Q: ch * Q + Q + 4]
    return np.ascontiguousarray(out.reshape(33, Q + 4, F // 128, 128).transpose(0, 3, 2, 1))

def ssd_params(inp, l, q):
    W = inp["w_in"][l]
    xs = 3840 + 256 * q; bs = 3840 + 1024 + 128 * q; cs = 3840 + 1536 + 128 * q
    wx = np.concatenate([W[:, xs:xs + 256], W[:, bs:bs + 128], W[:, cs:cs + 128]], 1)
    chans = np.concatenate([np.arange(256 * q, 256 * q + 256), np.arange(1024 + 128 * q, 1024 + 128 * q + 128),
                            np.arange(1536 + 128 * q, 1536 + 128 * q + 128)])
    cw = np.ascontiguousarray(inp["ssd_conv_w"][l][:, chans].T.reshape(4, 128, 4).transpose(1, 0, 2))
    cb = np.ascontiguousarray(inp["ssd_conv_b"][l][chans].reshape(4, 128).T)
    wdt = np.ascontiguousarray(W[:, 5888 + 4 * q:5888 + 4 * q + 4].reshape(8, 128, 4).transpose(1, 0, 2))
    alog = np.ascontiguousarray(np.broadcast_to(inp["ssd_a_log"][l][:, 4 * q:4 * q + 4], (128, 2, 4)))
    dtb = np.ascontiguousarray(np.broadcast_to(inp["ssd_dt_bias"][l][:, 4 * q:4 * q + 4], (128, 2, 4)))
    dsk = np.ascontiguousarray(np.repeat(inp["ssd_d"][l][4 * q:4 * q + 4], 64).reshape(2, 128).T)
    masks = np.ascontiguousarray(np.stack([np.triu(np.ones((128, 128), np.float32)), np.tril(np.ones((128, 128), np.float32))], 1))
    return dict(w_xbc=wrel(wx), w_dt=wdt, sconv_w=cw, sconv_b=cb, alog=alog, dtb=dtb, sdskip=dsk)


SHAPES_MIX = dict(w_u=[2, 128, 8, 128], lamre=[128, 12], lamim=[128, 12], logdt=[128, 12], Bre=[128, 12, 128], Bim=[128, 12, 128],
                  CreT=[128, 12, 128], CimT=[128, 12, 128], dskip=[128, 2],
                  w_x=[2, 128, 8, 128], w_gt=[2, 128, 8, 128], lconv_w=[128, 2, 4], lconv_b=[128, 2], WG=[128, 8, 128], BG=[128, 8],
                  llam=[128, 4],
                  w_xbc=[4, 128, 8, 128], w_dt=[128, 8, 4], sconv_w=[128, 4, 4], sconv_b=[128, 4], alog=[128, 2, 4], dtb=[128, 2, 4],
                  sdskip=[128, 2])
SHAPES_C = dict(w_z=[8, 128, 8, 128], w_g=[24, 128, 8, 128], w_glu=[6, 128, 6, 128], w_bra=[8, 128, 6, 128], w_brb=[8, 128, 8, 128],
                w_brc=[8, 128, 8, 128], w_out=[8, 128, 8, 128], ssd_nw=[128, 8], w_mod=[48, 128, 8, 128], b_mod=[128, 48],
                nw=[128, 2, 8])
SHAPES_C0 = dict(w1=[22, 128, 8, 128], w3=[22, 128, 8, 128], w2=[8, 128, 22, 128], w_modn=[16, 128, 8, 128], b_modn=[128, 16], nwn=[128, 8])
SHAPES_C1 = dict(w1=[224, 128, 8, 128], w3=[224, 128, 8, 128], w2=[64, 128, 28, 128], wr=[128, 8, 8], br=[128, 8], fnw=[128, 8])
SHAPES_A0 = dict(w_modn=[16, 128, 8, 128], b_modn=[128, 16], nwn=[128, 8])
SHAPES_CONST = dict(cond=[128, 8, 2], iota=[128, 256], ident=[128, 128], masks=[128, 2, 128], sel=[8, 8, 128])


def build_fused():
    nc = bass.Bass("TRN2", target_bir_lowering=False)
    k = K(nc)
    qv = nc.partition_id() % 4

    def ins(shapes, suffix):
        return {n: k.dram(n + suffix, s, kind="ExternalInput") for n, s in shapes.items()}
    xT = k.dram("xT", [4, 128, 8, 576], kind="ExternalInput")
    CONST = ins(SHAPES_CONST, "")
    PA = dict(CONST); PA.update(ins(SHAPES_A0, "_a"))
    PM = []; PC = []
    for l in range(2):
        pm = dict(CONST); pm.update(ins(SHAPES_MIX, f"_m{l}")); PM.append(pm)
        pc = dict(CONST); pc.update(ins(SHAPES_C, f"_c{l}")); pc.update(ins(SHAPES_C0 if l == 0 else SHAPES_C1, f"_c{l}")); PC.append(pc)
    oT = k.dram("oT", [4, 128, 8, 512], kind="ExternalOutput")
    hblk = [k.dram(f"hblk{i}", [4, 128, 8, 576]) for i in range(2)]
    xblk = k.dram("xblk", [4, 128, 8, 576])
    hall = k.dram("hall", [4, 4, 4, 32, 8, 576])
    ymix = k.dram("ymix", [3, 17, 128, 2, 512])
    yall = k.dram("yall", [3, 17, 4, 128, 2, 512])
    ystage = [k.dram(f"ystage{i}", [3, 4, 128, 2, 512]) for i in range(2)]
    for b_ in ystage:
        b_.track = True

    def gather(pieces):
        k.phase_begin("G")
        sem = k._sem()
        for (src_ap, dst_ap) in pieces:
            sb_ = Buf(src_ap, "gsrc"); sb_.dram = True
            db_ = Buf(dst_ap, "gdst"); db_.dram = True
            k.collective("AllGather", sb_, db_, GROUPS, sem=sem)
        k.phase_end()

    import os
    stop = int(os.environ.get("KF_STOP", "99"))
    dbg = os.environ.get("KF_DEBUG")

    def dcopy(name, src):
        d = k.dram(name, list(src.ap.shape), kind="ExternalOutput")
        k.phase_begin("D")
        k.dma(d, d.ap.opt() if False else d.ap, src, src.ap)
        k.phase_end()
    cc_h = k.cc_sem("cc_h"); cc_y = k.cc_sem("cc_y")
    phase_C(k, PA, False, None, True, "h_next", xT, None, None, qv, hblk[0], None, None, hgather=(hall, cc_h))
    for l in range(2):
        if l == 1:
            k.renew_engine_sems()
        k.cc_wait(cc_h)
        for m_, ph in enumerate((phase_S5, phase_LRU, phase_SSD)):
            ph(k, PM[l], hall, ymix)
            k.gather_async([(ymix.ap[m_, t_], yall.ap[m_, t_]) for t_ in range(17)], cc_y)
        k.cc_wait(cc_y)
        if l == 0:
            phase_C(k, PC[0], True, "dense", True, "h_next", xT, hblk[0], yall, qv, hblk[1], xblk, None, ystage, hgather=(hall, cc_h))
        else:
            k.renew_engine_sems()
            phase_C(k, PC[1], True, "moe", False, "final", xblk, hblk[1], yall, qv, None, None, oT, ystage)
    return nc


_NC_CACHE = {}


def kernel(**inp):
    inp = {k_: np.asarray(v, dtype=np.float32) for k_, v in inp.items()}
    x, c, ctx, c_ctx = inp["x"], inp["c"], inp["ctx"], inp["c_ctx"]
    cores = [(co // 4, co % 4) for co in range(8)]
    if "nc" not in _NC_CACHE:
        _NC_CACHE["nc"] = build_fused()
    nc = _NC_CACHE["nc"]
    sel = np.zeros((8, 8, 128), np.float32)
    for e_ in range(8):
        sel[e_, e_, :] = 1.0
    const = {"iota": np.ascontiguousarray(np.broadcast_to(np.arange(256, dtype=np.float32), (128, 256))),
             "ident": np.eye(128, dtype=np.float32), "sel": sel,
             "masks": np.ascontiguousarray(np.stack([np.triu(np.ones((128, 128), np.float32)), np.tril(np.ones((128, 128), np.float32))], 1))}
    shared = {}
    shared.update({"w_modn_a": wrel(inp["w_mod"][0][:, :2048]), "b_modn_a": vrel(inp["b_mod"][0][:2048]), "nwn_a": vrel(inp["norm_w"][0, 0])})
    for l in range(2):
        W_in = inp["w_in"][l]
        cw = {"w_z": wrel(W_in[:, 2816:3840]), "w_g": wrel(W_in[:, 5904:8976]), "w_glu": wrel(inp["s5_w_glu"][l]),
              "w_bra": wrel(inp["w_br_a"][l]), "w_brb": wrel(inp["w_br_b"][l]), "w_brc": wrel(inp["w_br_c"][l]),
              "w_out": wrel(inp["w_out"][l]), "ssd_nw": vrel(inp["ssd_norm_w"][l]), "w_mod": wrel(inp["w_mod"][l]),
              "b_mod": vrel(inp["b_mod"][l]), "nw": np.ascontiguousarray(inp["norm_w"][l].reshape(2, 8, 128).transpose(2, 0, 1))}
        if l == 0:
            cw.update({"w1": wrel(inp["ffn_w1"][0]), "w3": wrel(inp["ffn_w3"][0]), "w2": wrel(inp["ffn_w2"][0]),
                       "w_modn": wrel(inp["w_mod"][1][:, :2048]), "b_modn": vrel(inp["b_mod"][1][:2048]), "nwn": vrel(inp["norm_w"][1, 0])})
        else:
            cw.update({"w1": np.concatenate([wrel(inp["moe_w1"][0, e_]) for e_ in range(8)], 0),
                       "w3": np.concatenate([wrel(inp["moe_w3"][0, e_]) for e_ in range(8)], 0),
                       "w2": np.concatenate([wrel(inp["moe_w2"][0, e_]) for e_ in range(8)], 0),
                       "wr": np.ascontiguousarray(inp["moe_w_router"][0].reshape(8, 128, 8).transpose(1, 0, 2)),
                       "br": np.ascontiguousarray(np.broadcast_to(inp["moe_b_router"][0], (128, 8))), "fnw": vrel(inp["final_norm_w"])})
        shared.update({n + f"_c{l}": v for n, v in cw.items()})
    maps = []
    for (b, q) in cores:
        m = dict(const); m.update(shared)
        m["xT"] = fm_blocks(x[b, q * 2048:(q + 1) * 2048], ctx[b])
        m["cond"] = np.ascontiguousarray(np.stack([vrel(c[b]), vrel(c_ctx)], -1))
        for l in range(2):
            pm = {}
            pm.update(s5_params(inp, l, q)); pm.update(lru_params(inp, l, q)); pm.update(ssd_params(inp, l, q))
            m.update({n + f"_m{l}": v for n, v in pm.items()})
        maps.append(m)
    res = run_bass_kernel_spmd(nc, maps, core_ids=list(range(8))).results
    _NC_CACHE["res"] = res
    out = np.zeros((2, 8192, 1024), np.float32)
    for co, (b, q) in enumerate(cores):
        la, _ = un_blocks(res[co]["oT"], False)
        out[b, q * 2048:(q + 1) * 2048] = la
    return out
```

```python
import math
import os
from contextlib import ExitStack
import numpy as np
import concourse.bass as bass
import concourse.mybir as mybir
from concourse.bass_utils import run_bass_kernel_spmd

F32 = mybir.dt.float32
F32R = mybir.dt.float32r
I32 = mybir.dt.int32
AF = mybir.ActivationFunctionType
ALU = mybir.AluOpType
AX = mybir.AxisListType

SAME_ENGINE_SYNC = True
EPS = 1e-6
D = 1024
GROUPS = [[0, 1, 2, 3], [4, 5, 6, 7]]


def R(ap):
    return ap.bitcast(F32R)


class Buf:
    def __init__(self, ap, name=""):
        self.ap = ap
        self.name = name
        self.w = None
        self.r = []
        self.dram = False
        self.wall = {}
        self.rd = []
        self.excl = None
        self.dsem = None

    def __getitem__(self, idx):
        return self.ap[idx]

    def views(self, n):
        return [Buf(self.ap[:, i, :], f"{self.name}_{i}") for i in range(n)]


class K:
    def __init__(self, nc):
        self.nc = nc
        self.engs = {}
        for name, h in (("pe", nc.tensor), ("dve", nc.vector), ("act", nc.scalar),
                        ("pool", nc.gpsimd), ("sp", nc.sync)):
            sem = nc.alloc_semaphore("s_" + name)
            self.engs[name] = dict(h=h, sem=sem, n=0, seen={}, name=name)
        self.sem_pool = []
        self.semval = {}
        self.phase_sems = []
        self.drams = []
        self.stack = None
        self.pname = ""
        self.nbuf = 0

    def phase_begin(self, name):
        self.stack = ExitStack()
        self.pname = name
        self.phase_sems = []

    def phase_end(self):
        toks = [(e["sem"], e["n"], e["name"] + "_bar") for e in self.engs.values()]
        toks += [(s, self.semval[id(s)], "dma") for s in self.phase_sems]
        for e in self.engs.values():
            self._wait(e, toks)
        self.stack.close()
        self.stack = None
        self.sem_pool.extend(self.phase_sems)
        self.phase_sems = []
        for b in self.drams:
            b.wall = {}
            b.rd = []
            b.dsem = None

    def renew_engine_sems(self):
        for name, e in self.engs.items():
            self.nbuf += 1
            self.old_sems = getattr(self, "old_sems", []) + [e["sem"]]
            e["sem"] = self.nc.alloc_semaphore(f"s_{name}_r{self.nbuf}")
            e["n"] = 0

    def _sem(self):
        if self.sem_pool:
            s = self.sem_pool.pop()
        else:
            s = self.nc.alloc_semaphore(f"d{len(self.semval)}")
            self.semval[id(s)] = 0
        self.phase_sems.append(s)
        return s

    def sb(self, shape, dtype=F32, name=None):
        self.nbuf += 1
        name = f"{self.pname}_{name or 'sb'}_{self.nbuf}"
        t = self.stack.enter_context(self.nc.sbuf_tensor(name, list(shape), dtype))
        return Buf(t.ap(), name)

    def ps(self, shape, dtype=F32, name=None):
        self.nbuf += 1
        name = f"{self.pname}_{name or 'ps'}_{self.nbuf}"
        t = self.stack.enter_context(self.nc.psum_tensor(name, list(shape), dtype))
        b = Buf(t.ap(), name)
        b.excl = Buf(None, name + "_bank")
        return b

    def psview(self, parent, ap, name):
        b = Buf(ap, name)
        b.excl = parent.excl
        return b

    def dram(self, name, shape, dtype=F32, kind="Internal"):
        t = self.nc.dram_tensor(name, list(shape), dtype, kind=kind)
        b = Buf(t.ap(), name)
        b.dram = True
        self.drams.append(b)
        return b

    def _wait(self, e, toks):
        need = {}
        for tok in toks:
            if tok is None:
                continue
            sem, val, en = tok
            if en == e["name"] and (not SAME_ENGINE_SYNC or en in ("pe", "sp")):
                continue
            if en == e["name"] + "_bar":
                continue
            kk = id(sem)
            if need.get(kk, (None, 0))[1] < val:
                need[kk] = (sem, val)
        for kk, (sem, val) in need.items():
            if e["seen"].get(kk, 0) < val:
                e["h"].wait_ge(sem, val)
                e["seen"][kk] = val

    def _deps(self, reads, writes):
        toks = []
        for b in reads:
            toks.append(b.w)
        for b in writes:
            toks.append(b.w)
            toks.extend(b.r)
        return toks

    def op(self, eng, fn, reads=(), writes=()):
        e = self.engs[eng]
        reads = list(reads)
        writes = list(writes)
        for b in reads + writes:
            if b.excl is not None and b.excl not in writes:
                writes.append(b.excl)
        self._wait(e, self._deps(reads, writes))
        e["n"] += 1
        fn(e["h"]).then_inc(e["sem"], 1)
        tok = (e["sem"], e["n"], eng)
        for b in reads:
            b.r.append(tok)
        for b in writes:
            b.w = tok
            b.r = []

    def dma(self, outs, out_ap, ins, in_ap, q="sp", **kw):
        e = self.engs[q]
        outs = [] if outs is None else (outs if isinstance(outs, (list, tuple)) else [outs])
        ins = [] if ins is None else (ins if isinstance(ins, (list, tuple)) else [ins])
        sb_in = [b for b in ins if not b.dram]
        owner = sb_in[0] if sb_in else outs[0]
        if owner.dsem is None:
            owner.dsem = self._sem()
        toks = []
        for b in ins:
            if b.dram:
                toks.extend(b.wall.values())
            else:
                toks.append(b.w)
        for b in outs:
            if b.dram:
                toks.extend(b.rd)
            else:
                if b.w is not None and b.w[0] is not owner.dsem:
                    toks.append(b.w)
                toks.extend(b.r)
        self._wait(e, toks)
        self.semval[id(owner.dsem)] += 16
        e["h"].dma_start(out=out_ap, in_=in_ap, **kw).then_inc(owner.dsem, 16)
        tok = (owner.dsem, self.semval[id(owner.dsem)], "dma")
        for b in sb_in:
            b.r.append(tok)
        for b in ins:
            if b.dram and getattr(b, "track", False):
                b.rd.append(tok)
        for b in outs:
            if b.dram:
                b.wall[id(owner.dsem)] = tok
            else:
                b.w = tok
                b.r = []

    def collective(self, kind, in_b, out_b, groups, sem=None):
        e = self.engs["pool"]
        toks = list(in_b.wall.values()) + list(out_b.wall.values()) + list(out_b.rd)
        self._wait(e, toks)
        sem = sem or self._sem()
        self.semval[id(sem)] += 1
        e["h"].collective_compute(kind, ALU.bypass, replica_groups=groups, ins=[in_b.ap.opt()],
                                  outs=[out_b.ap.opt()]).then_inc(sem, 1)
        tok = (sem, self.semval[id(sem)], "dma")
        out_b.wall[id(sem)] = tok
        in_b.rd.append(tok)

    def cc_sem(self, name):
        sem = self.nc.alloc_semaphore(name)
        self.semval[id(sem)] = 0
        self.keep = getattr(self, "keep", []) + [sem]
        return sem

    def gather_async(self, pieces, sem, toks=()):
        self._wait(self.engs["pool"], list(toks))
        for (src_ap, dst_ap) in pieces:
            sb_ = Buf(src_ap, "gsrc"); sb_.dram = True
            db_ = Buf(dst_ap, "gdst"); db_.dram = True
            self.collective("AllGather", sb_, db_, GROUPS, sem=sem)

    def cc_wait(self, sem):
        tok = (sem, self.semval[id(sem)], "dma")
        for e in self.engs.values():
            self._wait(e, [tok])

    def finish(self, bufs, eng="sp"):
        e = self.engs[eng]
        toks = []
        for b in bufs:
            toks.append(b.w)
            toks.extend(b.r)
            toks.extend(b.wall.values())
        self._wait(e, toks)


def nsplit(W):
    return [(0, min(W, 512))] + ([(512, W)] if W > 512 else [])


def linear(k, wd, mcs, KC, rhs, W, evac):
    for mc in mcs:
        wb = k.wbuf[k.wi % 2]
        k.wi += 1
        for k0 in range(0, KC, 8):
            n = min(8, KC - k0)
            st = k.wstg[k.si % len(k.wstg)]
            k.si += 1
            k.dma(st, st[:, 0:n, :], wd, wd.ap[mc][:, k0:k0 + n, :], q="sp")
            k.op("dve", lambda e, st=st, wb=wb, k0=k0, n=n: e.tensor_copy(out=wb[:, k0:k0 + n, :], in_=st[:, 0:n, :]),
                 reads=[st], writes=[wb])
        ps = k.psl[k.pi % len(k.psl)]
        k.pi += 1
        for (n0, n1) in nsplit(W):
            for kc in range(KC):
                rb = rhs[kc]
                k.op("pe", lambda e, ps=ps, wb=wb, rb=rb, kc=kc, n0=n0, n1=n1: e.matmul(
                    ps[:, n0:n1], wb[:, kc, :], R(rb[:, n0:n1]), start=(kc == 0), stop=(kc == KC - 1)),
                    reads=[wb, rb], writes=[ps])
        evac(mc, ps)


def load_h(k, hall, hv, hb, ch, off=0, zt=None):
    def src(r, bl, pg, c0, n):
        return hall.ap[bl, pg, r, :, :, c0:c0 + n]
    if ch == 0:
        for bl in range(4):
            c0 = off + bl * 64
            for pg in range(4):
                k.dma(hv, R(hb[32 * pg:32 * pg + 32, :, c0:c0 + 64]), hall, src(0, bl, pg, 512, 64), q="pool")
    else:
        t0 = (ch - 1) * 256
        r, bl, col = t0 // 2048, (t0 % 2048) // 512, t0 % 512
        for pg in range(4):
            k.dma(hv, R(hb[32 * pg:32 * pg + 32, :, off:off + 256]), hall, src(r, bl, pg, col, 256), q="pool")
    if zt is not None:
        for side in (0, 1):
            dst = hb[:, :, 0:2] if side == 0 else hb[:, :, off + 256:off + 258]
            tt = (ch - 1) * 256 - 2 if side == 0 else ch * 256
            if ch == 0 or tt < 0 or tt >= 8192:
                k.op("act", lambda e, dst=dst: e.activation(out=R(dst), in_=zt[:, :, 0:2], func=AF.Copy), reads=[zt], writes=hv)
            else:
                r, bl, col = tt // 2048, (tt % 2048) // 512, tt % 512
                d0 = 0 if side == 0 else off + 256
                for pg in range(4):
                    k.dma(hv, R(hb[32 * pg:32 * pg + 32, :, d0:d0 + 2]), hall, src(r, bl, pg, col, 2), q="pool")


def rmsnorm(k, X, W, segs, scale_t, shift_t, out, f32r, sqs, ones, shift_b=None):
    ps = k.psl[k.pi % len(k.psl)]
    k.pi += 1
    for c in range(8):
        sq = sqs[c % 2]
        k.op("act", lambda e, sq=sq, c=c: e.activation(out=R(sq[:, 0:W]), in_=X[c][:, 0:W], func=AF.Square),
             reads=[X[c]], writes=[sq])
        for (n0, n1) in nsplit(W):
            k.op("pe", lambda e, sq=sq, c=c, n0=n0, n1=n1: e.matmul(
                ps[:, n0:n1], ones[:, :], R(sq[:, n0:n1]), start=(c == 0), stop=(c == 7)),
                reads=[sq, ones], writes=[ps])
    rstd = k.rstd
    k.op("act", lambda e: e.activation(out=rstd[:, 0:W], in_=ps[:, 0:W], func=AF.Sqrt, scale=1.0 / D, bias=k.epsb[:, 0:1]),
         reads=[ps, k.epsb], writes=[rstd])
    k.op("dve", lambda e: e.reciprocal(out=rstd[:, 0:W], in_=rstd[:, 0:W]), reads=[rstd], writes=[rstd])
    for c in range(8):
        for (c0, c1, j) in segs:
            o = out[c][:, c0:c1]
            if f32r:
                o = R(o)
            if shift_t is None:
                k.op("dve", lambda e, o=o, c=c, c0=c0, c1=c1, j=j: e.scalar_tensor_tensor(
                    out=o, in0=X[c][:, c0:c1], scalar=scale_t[:, c, j:j + 1], in1=rstd[:, c0:c1],
                    op0=ALU.mult, op1=ALU.mult), reads=[X[c], scale_t, rstd], writes=[out[c]])
            else:
                tmp = k.tmp[c % 2]
                k.op("dve", lambda e, tmp=tmp, c=c, c0=c0, c1=c1, j=j: e.scalar_tensor_tensor(
                    out=tmp[:, c0:c1], in0=X[c][:, c0:c1], scalar=scale_t[:, c, j:j + 1], in1=rstd[:, c0:c1],
                    op0=ALU.mult, op1=ALU.mult), reads=[X[c], scale_t, rstd], writes=[tmp])
                k.op("act", lambda e, o=o, tmp=tmp, c=c, c0=c0, c1=c1, j=j: e.activation(
                    out=o, in_=tmp[:, c0:c1], func=AF.Identity, bias=shift_t[:, c, j:j + 1], scale=1.0),
                    reads=[tmp, shift_b], writes=[out[c]])


def phase_C(k, P, has_mix, ffn, has_ctx, tail, xsrc, hsrc, yall, qv, hdst, xdst, oT, ystage=None, hgather=None, NB=4):
    k.phase_begin("C")
    W = 576 if has_ctx else 512
    segs = [(0, 512, 0)] + ([(512, 576, 1)] if has_ctx else [])
    xT = xsrc
    if has_mix:
        hT = hsrc
        w_z, w_g, w_glu = P["w_z"], P["w_g"], P["w_glu"]
        w_bra, w_brb, w_brc, w_out, ssd_nw = P["w_bra"], P["w_brb"], P["w_brc"], P["w_out"], P["ssd_nw"]
    if has_mix or ffn:
        w_mod, b_mod, nw = P["w_mod"], P["b_mod"], P["nw"]
    cond = P["cond"]
    if ffn == "dense":
        w1, w3, w2 = P["w1"], P["w3"], P["w2"]
        NH = 22
    elif ffn == "moe":
        w1, w3, w2 = P["w1"], P["w3"], P["w2"]
        wr, br, sel, ident = P["wr"], P["br"], P["sel"], P["ident"]
        NH = 28
    else:
        NH = 0
    if tail == "h_next":
        w_modn, b_modn, nwn = P["w_modn"], P["b_modn"], P["nwn"]
        hT_o, xT_o = hdst, xdst
    else:
        fnw = P["fnw"]

    k.wbuf = [k.sb([128, 28, 128], F32R, name=f"wb{i}") for i in range(2)]
    k.wstg = [k.sb([128, 8, 128], name=f"wstg{i}") for i in range(4)]
    k.si = 0
    k.wi = 0
    k.psl = [k.ps([128, 1024], name=f"psl{i}") for i in range(3)]
    k.pi = 0
    psx = k.ps([128, 1024], name="psx")
    Xt = k.sb([128, 8, W], name="X"); X = Xt.views(8)
    Ht = k.sb([128, 8, W], name="H"); H = Ht.views(8)
    k.rstd = k.sb([128, W], name="rstd")
    k.tmp = [k.sb([128, W], name=f"tmp{i}") for i in range(2)]
    sqs = [k.sb([128, W], name=f"sq{i}") for i in range(2)]
    k.acc = k.sb([128, W], name="accs")
    OUTt, OUT = Xt, X
    ones32 = k.sb([128, 128], name="ones32")
    k.op("dve", lambda e: e.memset(ones32[:, :], 1.0), writes=[ones32])
    ones = k.sb([128, 128], F32R, name="ones")
    k.op("act", lambda e: e.activation(out=ones[:, :], in_=ones32[:, :], func=AF.Copy), reads=[ones32], writes=[ones])
    k.epsb = k.sb([128, 1], name="epsb")
    k.op("dve", lambda e: e.memset(k.epsb[:, :], EPS), writes=[k.epsb])
    if has_mix or ffn:
        MGt = k.sb([128, 8, W], name="MG"); MG = MGt.views(8)
        St = k.sb([128, max(30, NH), W], name="S"); S = St.views(max(30, NH))
        YG, YB, YC, Z = S[0:6], S[6:14], S[14:22], S[22:30]
        HID = S

    condt = k.sb([128, 8, 2], name="condt")
    k.dma(condt, condt[:], cond, cond[:])
    conds_t = k.sb([128, 8, 2], name="conds"); conds = conds_t.views(8)
    for c in range(8):
        k.op("act", lambda e, c=c: e.activation(out=R(conds[c][:, :]), in_=condt[:, c, :], func=AF.Silu),
             reads=[condt], writes=[conds[c]])

    def mod_vectors(wd, bd, nmc, name):
        bt = k.sb([128, nmc], name=name + "_b")
        k.dma(bt, bt[:], bd, bd[:])
        mt = k.sb([128, nmc, 2], name=name)

        def ev(mc, ps):
            k.op("dve", lambda e, mc=mc, ps=ps: e.tensor_scalar(out=mt[:, mc, :], in0=ps[:, 0:2], scalar1=bt[:, mc:mc + 1],
                                                               scalar2=None, op0=ALU.add), reads=[ps, bt], writes=[mt])
        linear(k, wd, range(nmc), 8, conds, 2, ev)
        return mt

    def scale_of(mt, off, nwt, nwi, name):
        st = k.sb([128, 8, 2], name=name)
        for j in range(2):
            k.op("dve", lambda e, j=j: e.scalar_tensor_tensor(out=st[:, :, j], in0=mt[:, off:off + 8, j], scalar=1.0,
                                                              in1=nwi, op0=ALU.add, op1=ALU.mult),
                 reads=[mt, nwt], writes=[st])
        return st

    if has_mix or ffn:
        MOD = mod_vectors(w_mod, b_mod, 48, "MOD")
        nwt = k.sb([128, 2, 8], name="nwt")
        k.dma(nwt, nwt[:], nw, nw[:])
        A4 = scale_of(MOD, 32, nwt, nwt[:, 1, :], "A4")
    if tail == "h_next":
        MODN = mod_vectors(w_modn, b_modn, 16, "MODN")
        nwnt = k.sb([128, 8], name="nwnt")
        k.dma(nwnt, nwnt[:], nwn, nwn[:])
        A1N = scale_of(MODN, 8, nwnt, nwnt[:, :], "A1N")
    else:
        fnwt = k.sb([128, 8, 1], name="fnwt")
        k.dma(fnwt, fnwt[:, :, 0], fnw, fnw[:])
    if has_mix:
        snw = k.sb([128, 8, 1], name="snw")
        k.dma(snw, snw[:, :, 0], ssd_nw, ssd_nw[:])
    if ffn == "moe":
        wrt = k.sb([128, 8, 8], F32R, name="wrt")
        k.dma(wrt, wrt[:], wr, wr[:], q="pool")
        brt = k.sb([128, 8], name="brt")
        k.dma(brt, brt[:], br, br[:])
        selt = k.sb([8, 8, 128], F32R, name="selt")
        k.dma(selt, selt[:], sel, sel[:], q="pool")
        idt = k.sb([128, 128], name="idt")
        k.dma(idt, idt[:], ident, ident[:])
        GT = k.sb([8, W], name="GT")
        GBt = k.sb([128, 8, W], name="GB"); GB = GBt.views(8)
        L = k.sb([128, 8], name="L"); M8 = k.sb([128, 8], name="M8"); msk = k.sb([128, 8], name="msk")
        ex = k.sb([128, 8], name="ex"); sc1 = k.sb([128, 2], name="sc1")

    def resid(mc, ps, moff):
        for (c0, c1, j) in segs:
            k.op("dve", lambda e, c0=c0, c1=c1, j=j: e.scalar_tensor_tensor(
                out=X[mc][:, c0:c1], in0=ps[:, c0:c1], scalar=MOD[:, moff + mc, j:j + 1], in1=X[mc][:, c0:c1],
                op0=ALU.mult, op1=ALU.add), reads=[ps, MOD, X[mc]], writes=[X[mc]])

    for blk in range(NB):
        k.dma(X, Xt[:], xT, xT.ap[blk][:, :, 0:W])
        if has_mix:
            k.dma(H, R(Ht[:]), hT, hT.ap[blk][:, :, 0:W], q="pool")
            def stage(b_):
                ys_ = ystage[b_ % 2]
                k.dma(ys_, ys_.ap.rearrange("m r p c w -> m (r p c w)"), yall,
                      yall.ap[:, bass.ds(qv * 4 + b_, 1)].rearrange("m o r p c w -> m (o r p c w)"), q="pool")
            if blk == 0:
                stage(0)
            yst = ystage[blk % 2]
            def ld_y(dst_bufs, dst_t, c_dst, p0, np_, r, m, c_src, ps0):
                k.dma(dst_bufs, R(dst_t[p0:p0 + np_, c_dst, 0:512]), yst, yst.ap[m, r, ps0:ps0 + np_, c_src, :], q="pool")
                if has_ctx:
                    k.dma(dst_bufs, R(dst_t[p0:p0 + np_, c_dst, 512:576]), yall, yall.ap[m, 16, r, ps0:ps0 + np_, c_src, blk * 64:blk * 64 + 64], q="pool")
            for gt_ in range(6):
                g0 = gt_ * 128
                while g0 < gt_ * 128 + 128:
                    r = g0 // 192
                    lc = g0 - 192 * r
                    c_src, ps0 = lc // 128, lc % 128
                    n_ = min(128 - ps0, gt_ * 128 + 128 - g0, 192 - lc)
                    ld_y([YG[gt_]], St, gt_, g0 - gt_ * 128, n_, r, 0, c_src, ps0)
                    g0 += n_
            for gt_ in range(8):
                ld_y([YB[gt_]], St, 6 + gt_, 0, 128, gt_ // 2, 1, gt_ % 2, 0)
                ld_y([YC[gt_]], St, 14 + gt_, 0, 128, gt_ // 2, 2, gt_ % 2, 0)
            if blk + 1 < NB:
                stage(blk + 1)
            if os.environ.get("KF_DEBUG") and blk == 1:
                dS = k.dram("dbg_S" if has_ctx else "dbg_S1", [128, 22, W], kind="ExternalOutput")
                k.dma(dS, dS.ap, S[0:22], St[:, 0:22, :], q="act")
                dX = k.dram("dbg_X" if has_ctx else "dbg_X1", [128, 16, W], kind="ExternalOutput")
                k.dma(dX, dX.ap[:, 0:8, :], X, Xt[:], q="act")
                k.dma(dX, dX.ap[:, 8:16, :], H, Ht[:], q="act")
            def ev_glu(mc, ps):
                t = k.tmp[mc % 2]
                k.op("act", lambda e, t=t, ps=ps: e.activation(out=t[:, 0:W], in_=ps[:, 0:W], func=AF.Sigmoid),
                     reads=[ps], writes=[t])
                k.op("dve", lambda e, t=t, mc=mc: e.tensor_tensor(out=R(MG[mc][:, 0:W]), in0=t[:, 0:W],
                                                                   in1=YG[mc][:, 0:W].bitcast(F32), op=ALU.mult),
                     reads=[t, YG[mc]], writes=[MG[mc]])
            linear(k, w_glu, range(6), 6, YG, W, ev_glu)
            YA = MG[0:6]
            def ev_z(mc, ps):
                t = k.tmp[mc % 2]
                k.op("act", lambda e, t=t, ps=ps: e.activation(out=t[:, 0:W], in_=ps[:, 0:W], func=AF.Silu),
                     reads=[ps], writes=[t])
                k.op("dve", lambda e, t=t, mc=mc: e.tensor_tensor(out=R(YC[mc][:, 0:W]), in0=t[:, 0:W], in1=YC[mc][:, 0:W],
                                                                   op=ALU.mult), reads=[t, YC[mc]], writes=[YC[mc]])
            linear(k, w_z, range(8), 8, H, W, ev_z)
            rmsnorm(k, YC, W, [(0, W, 0)], snw, None, Z, True, sqs, ones)
            YCN = Z
            MRG = YC
            for mc in range(8):
                first = [True]
                for bi, (wbr, KCb, src) in enumerate(((w_bra, 6, YA), (w_brb, 8, YB), (w_brc, 8, YCN))):
                    gt = k.tmp[bi % 2]

                    def ev_gate(_mc, ps, gt=gt):
                        k.op("act", lambda e, ps=ps, gt=gt: e.activation(out=gt[:, 0:W], in_=ps[:, 0:W], func=AF.Sigmoid),
                             reads=[ps], writes=[gt])
                    linear(k, w_g, [bi * 8 + mc], 8, H, W, ev_gate)

                    def ev_br(_mc, ps, gt=gt, bi=bi, mc=mc):
                        if bi == 0:
                            k.op("dve", lambda e, ps=ps, gt=gt, mc=mc: e.tensor_tensor(
                                out=k.acc[:, 0:W], in0=ps[:, 0:W], in1=gt[:, 0:W], op=ALU.mult),
                                reads=[ps, gt], writes=[k.acc])
                        else:
                            k.op("dve", lambda e, ps=ps, gt=gt, mc=mc: e.tensor_tensor(
                                out=gt[:, 0:W], in0=ps[:, 0:W], in1=gt[:, 0:W], op=ALU.mult),
                                reads=[ps, gt], writes=[gt])
                            o = R(MRG[mc][:, 0:W]) if bi == 2 else k.acc[:, 0:W]
                            ob = MRG[mc] if bi == 2 else k.acc
                            k.op("dve", lambda e, gt=gt, o=o: e.tensor_tensor(
                                out=o, in0=gt[:, 0:W], in1=k.acc[:, 0:W], op=ALU.add),
                                reads=[gt, k.acc], writes=[ob])
                    linear(k, wbr, [mc], KCb, src, W, ev_br)
            linear(k, w_out, range(8), 8, MRG, W, lambda mc, ps: resid(mc, ps, 16))
        if ffn:
            rmsnorm(k, X, W, segs, A4, MOD[:, 24:32, :], H, True, sqs, ones, shift_b=MOD)
            if ffn == "dense":
                experts = [0]
            else:
                experts = list(range(8))
                for sb_ in range(W // 128):
                    t0 = sb_ * 128
                    for kc in range(8):
                        k.op("pe", lambda e, kc=kc, t0=t0: e.matmul(psx[:, 0:8], R(H[kc][:, t0:t0 + 128]), wrt[:, kc, :],
                                                                    start=(kc == 0), stop=(kc == 7)),
                             reads=[H[kc], wrt], writes=[psx])
                    k.op("dve", lambda e: e.tensor_tensor(out=L[:, :], in0=psx[:, 0:8], in1=brt[:, :], op=ALU.add),
                         reads=[psx, brt], writes=[L])
                    k.op("dve", lambda e: e.max(out=M8[:, :], in_=L[:, :]), reads=[L], writes=[M8])
                    k.op("dve", lambda e: e.tensor_scalar(out=msk[:, :], in0=L[:, :], scalar1=M8[:, 1:2], scalar2=None,
                                                          op0=ALU.is_ge), reads=[L, M8], writes=[msk])
                    k.op("dve", lambda e: e.tensor_scalar(out=sc1[:, 0:1], in0=M8[:, 0:1], scalar1=-1.0, scalar2=None,
                                                          op0=ALU.mult), reads=[M8], writes=[sc1])
                    k.op("act", lambda e: e.activation(out=ex[:, :], in_=L[:, :], func=AF.Exp, bias=sc1[:, 0:1], scale=1.0),
                         reads=[L, sc1], writes=[ex])
                    k.op("dve", lambda e: e.tensor_tensor(out=ex[:, :], in0=ex[:, :], in1=msk[:, :], op=ALU.mult),
                         reads=[ex, msk], writes=[ex])
                    k.op("dve", lambda e: e.reduce_sum(out=sc1[:, 1:2], in_=ex[:, :], axis=AX.X), reads=[ex], writes=[sc1])
                    k.op("dve", lambda e: e.reciprocal(out=sc1[:, 1:2], in_=sc1[:, 1:2]), reads=[sc1], writes=[sc1])
                    k.op("dve", lambda e: e.tensor_scalar(out=ex[:, :], in0=ex[:, :], scalar1=sc1[:, 1:2], scalar2=None,
                                                          op0=ALU.mult), reads=[ex, sc1], writes=[ex])
                    k.op("pe", lambda e: e.transpose(out=psx[0:8, 512:640], in_=ex[:, :], identity=idt[:, :]),
                         reads=[ex, idt], writes=[psx])
                    k.op("act", lambda e, t0=t0: e.activation(out=R(GT[:, t0:t0 + 128]), in_=psx[0:8, 512:640], func=AF.Copy),
                         reads=[psx], writes=[GT])
                for ex_i in range(8):
                    k.op("pe", lambda e, ex_i=ex_i: e.matmul(psx[:, 0:W], selt[:, ex_i, :], R(GT[:, 0:W]), start=True, stop=True),
                         reads=[selt, GT], writes=[psx])
                    k.op("act", lambda e, ex_i=ex_i: e.activation(out=GB[ex_i][:, 0:W], in_=psx[:, 0:W], func=AF.Copy),
                         reads=[psx], writes=[GB[ex_i]])
            for ei in experts:
                for mc in range(NH):
                    t = k.tmp[mc % 2]

                    def ev1(_mc, ps, t=t):
                        k.op("act", lambda e, ps=ps, t=t: e.activation(out=t[:, 0:W], in_=ps[:, 0:W], func=AF.Silu),
                             reads=[ps], writes=[t])
                    linear(k, w1, [ei * NH + mc], 8, H, W, ev1)

                    def ev3(_mc, ps, t=t, mc=mc, ei=ei):
                        if ffn == "dense":
                            k.op("dve", lambda e, ps=ps, t=t, mc=mc: e.tensor_tensor(
                                out=R(HID[mc][:, 0:W]), in0=ps[:, 0:W], in1=t[:, 0:W], op=ALU.mult),
                                reads=[ps, t], writes=[HID[mc]])
                        else:
                            k.op("dve", lambda e, ps=ps, t=t: e.tensor_tensor(
                                out=t[:, 0:W], in0=ps[:, 0:W], in1=t[:, 0:W], op=ALU.mult), reads=[ps, t], writes=[t])
                            k.op("pool", lambda e, t=t, mc=mc, ei=ei: e.tensor_tensor(
                                out=R(HID[mc][:, 0:W]), in0=t[:, 0:W], in1=GB[ei][:, 0:W], op=ALU.mult),
                                reads=[t, GB[ei]], writes=[HID[mc]])
                    linear(k, w3, [ei * NH + mc], 8, H, W, ev3)
                for mc in range(8):
                    linear(k, w2, [ei * 8 + mc], NH, HID, W, lambda _mc, ps, mc=mc: resid(mc, ps, 40))
        if tail == "h_next":
            if xT_o is not None:
                k.dma(xT_o, xT_o.ap[blk][:, :, 0:W], X, Xt[:], q="act")
            rmsnorm(k, X, W, segs, A1N, MODN[:, 0:8, :], OUT, False, sqs, ones, shift_b=MODN)
            k.dma(hT_o, hT_o.ap[blk][:, :, 0:W], OUT, OUTt[:], q="act")
            if hgather is not None:
                hall_, sem_ = hgather
                k.gather_async([(hT_o.ap[blk, 32 * pg_:32 * pg_ + 32], hall_.ap[blk, pg_]) for pg_ in range(4)], sem_,
                               toks=list(hT_o.wall.values()))
        else:
            rmsnorm(k, X, W, [(0, W, 0)], fnwt, None, OUT, False, sqs, ones)
            k.dma(oT, oT[blk], OUT, OUTt[:], q="act")
    k.phase_end()


Q = 256
NCH = 33
TWO_PI = 6.283185
HW_ = 260

def ydst(ymix, m, ch):
    if ch == 0:
        return ymix.ap[m, 16, :, :, 0:256]
    return ymix.ap[m, (ch - 1) // 2, :, :, ((ch - 1) % 2) * 256:((ch - 1) % 2) * 256 + 256]

def phase_S5(k, P, hall, ymix):
    k.phase_begin("S5")
    w_u = P["w_u"]
    lamre, lamim, logdt = P["lamre"], P["lamim"], P["logdt"]
    Bre, Bim, CreT, CimT = P["Bre"], P["Bim"], P["CreT"], P["CimT"]
    dskip, iota, ident = P["dskip"], P["iota"], P["ident"]

    k.wbuf = [k.sb([128, 8, 128], F32R, name=f"wb{i}") for i in range(2)]
    k.wstg = [k.sb([128, 8, 128], name=f"wstg{i}") for i in range(4)]
    k.si = 0
    k.wi = 0
    k.psl = [k.ps([128, 512], name=f"psu{i}") for i in range(2)]
    k.pi = 0
    psb = [k.ps([128, 512], name=f"psb{i}") for i in range(4)]
    psy = [k.ps([128, 512], name=f"psy{i}") for i in range(2)]

    def small(name, shape=(128, 12)):
        return k.sb(list(shape), name=name)

    def ld(name, src, shape, q="sp", dt=F32):
        t = k.sb(list(shape), dt, name=name)
        k.dma(t, t[:], src, src[:], q=q)
        return t

    lre = ld("lre", lamre, [128, 12]); lim = ld("lim", lamim, [128, 12]); ldt = ld("ldt", logdt, [128, 12])
    bre = ld("bre", Bre, [128, 12, 128]); bim = ld("bim", Bim, [128, 12, 128])
    cre = ld("cre", CreT, [128, 12, 128], q="pool", dt=F32R); cim = ld("cim", CimT, [128, 12, 128], q="pool", dt=F32R)
    dsk = ld("dsk", dskip, [128, 2]); iot = ld("iot", iota, [128, Q]); idt = ld("idt", ident, [128, 128])

    V = lambda eng, fn, r, w: k.op(eng, fn, reads=r, writes=w)
    dt_ = small("dt"); rr = small("rr"); thn = small("thn")
    V("act", lambda e: e.activation(out=dt_[:, :], in_=ldt[:, :], func=AF.Exp), [ldt], [dt_])
    V("dve", lambda e: e.tensor_tensor(out=rr[:, :], in0=lre[:, :], in1=dt_[:, :], op=ALU.mult), [lre, dt_], [rr])
    V("act", lambda e: e.activation(out=rr[:, :], in_=rr[:, :], func=AF.Exp), [rr], [rr])
    V("dve", lambda e: e.scalar_tensor_tensor(out=thn[:, :], in0=lim[:, :], scalar=1.0 / (2 * math.pi), in1=dt_[:, :],
                                              op0=ALU.mult, op1=ALU.mult), [lim, dt_], [thn])
    cosT = k.sb([128, 12, Q], name="cosT"); sinT = k.sb([128, 12, Q], name="sinT")
    xf = k.sb([128, Q], name="xf"); xi = k.sb([128, Q], I32, name="xi"); xr = k.sb([128, Q], name="xr")
    mk = k.sb([128, Q], name="mk")

    def wrap(f):
        V("dve", lambda e: e.tensor_single_scalar(out=mk[:, :], in_=f[:, :], scalar=0.5, op=ALU.is_gt), [f], [mk])
        V("dve", lambda e: e.tensor_tensor(out=f[:, :], in0=f[:, :], in1=mk[:, :], op=ALU.subtract), [f, mk], [f])
        V("dve", lambda e: e.tensor_single_scalar(out=mk[:, :], in_=f[:, :], scalar=-0.5, op=ALU.is_lt), [f], [mk])
        V("dve", lambda e: e.tensor_tensor(out=f[:, :], in0=f[:, :], in1=mk[:, :], op=ALU.add), [f, mk], [f])

    for di in range(12):
        V("dve", lambda e, di=di: e.tensor_scalar(out=xf[:, :], in0=iot[:, :], scalar1=thn[:, di:di + 1], scalar2=None,
                                                  op0=ALU.mult), [iot, thn], [xf])
        V("dve", lambda e: e.tensor_copy(out=xi[:, :], in_=xf[:, :]), [xf], [xi])
        V("dve", lambda e: e.tensor_copy(out=xr[:, :], in_=xi[:, :]), [xi], [xr])
        V("dve", lambda e: e.tensor_tensor(out=xf[:, :], in0=xf[:, :], in1=xr[:, :], op=ALU.subtract), [xf, xr], [xf])
        wrap(xf)
        V("act", lambda e, di=di: e.activation(out=sinT[:, di, :], in_=xf[:, :], func=AF.Sin, scale=TWO_PI), [xf], [sinT])
        V("dve", lambda e: e.tensor_single_scalar(out=xf[:, :], in_=xf[:, :], scalar=0.25, op=ALU.add), [xf], [xf])
        wrap(xf)
        V("act", lambda e, di=di: e.activation(out=cosT[:, di, :], in_=xf[:, :], func=AF.Sin, scale=TWO_PI), [xf], [cosT])
    ar = small("ar"); ai = small("ai"); nr = small("nr"); ni = small("ni"); den = small("den"); t12 = small("t12")
    V("dve", lambda e: e.tensor_tensor(out=ar[:, :], in0=rr[:, :], in1=cosT[:, :, 1], op=ALU.mult), [rr, cosT], [ar])
    V("dve", lambda e: e.tensor_single_scalar(out=ar[:, :], in_=ar[:, :], scalar=-1.0, op=ALU.add), [ar], [ar])
    V("dve", lambda e: e.tensor_tensor(out=ai[:, :], in0=rr[:, :], in1=sinT[:, :, 1], op=ALU.mult), [rr, sinT], [ai])
    V("dve", lambda e: e.tensor_tensor(out=nr[:, :], in0=ar[:, :], in1=lre[:, :], op=ALU.mult), [ar, lre], [nr])
    V("dve", lambda e: e.tensor_tensor(out=t12[:, :], in0=ai[:, :], in1=lim[:, :], op=ALU.mult), [ai, lim], [t12])
    V("dve", lambda e: e.tensor_tensor(out=nr[:, :], in0=nr[:, :], in1=t12[:, :], op=ALU.add), [nr, t12], [nr])
    V("dve", lambda e: e.tensor_tensor(out=ni[:, :], in0=ai[:, :], in1=lre[:, :], op=ALU.mult), [ai, lre], [ni])
    V("dve", lambda e: e.tensor_tensor(out=t12[:, :], in0=ar[:, :], in1=lim[:, :], op=ALU.mult), [ar, lim], [t12])
    V("dve", lambda e: e.tensor_tensor(out=ni[:, :], in0=ni[:, :], in1=t12[:, :], op=ALU.subtract), [ni, t12], [ni])
    V("dve", lambda e: e.tensor_tensor(out=den[:, :], in0=lre[:, :], in1=lre[:, :], op=ALU.mult), [lre], [den])
    V("dve", lambda e: e.tensor_tensor(out=t12[:, :], in0=lim[:, :], in1=lim[:, :], op=ALU.mult), [lim], [t12])
    V("dve", lambda e: e.tensor_tensor(out=den[:, :], in0=den[:, :], in1=t12[:, :], op=ALU.add), [den, t12], [den])
    V("dve", lambda e: e.reciprocal(out=den[:, :], in_=den[:, :]), [den], [den])
    V("dve", lambda e: e.tensor_tensor(out=nr[:, :], in0=nr[:, :], in1=den[:, :], op=ALU.mult), [nr, den], [nr])
    V("dve", lambda e: e.tensor_tensor(out=ni[:, :], in0=ni[:, :], in1=den[:, :], op=ALU.mult), [ni, den], [ni])
    breT = k.sb([128, 12, 128], F32R, name="breT"); bimT = k.sb([128, 12, 128], F32R, name="bimT")
    tb1 = k.sb([128, 128], name="tb1"); tb2 = k.sb([128, 128], name="tb2")
    for di in range(12):
        V("dve", lambda e, di=di: e.tensor_scalar(out=tb1[:, :], in0=bim[:, di, :], scalar1=ni[:, di:di + 1], scalar2=None,
                                                  op0=ALU.mult), [bim, ni], [tb1])
        V("dve", lambda e, di=di: e.scalar_tensor_tensor(out=tb1[:, :], in0=bre[:, di, :], scalar=nr[:, di:di + 1],
                                                         in1=tb1[:, :], op0=ALU.mult, op1=ALU.subtract), [bre, nr, tb1], [tb1])
        V("pe", lambda e: e.transpose(out=psb[0][:, 0:128], in_=tb1[:, :], identity=idt[:, :]), [tb1, idt], [psb[0]])
        V("act", lambda e, di=di: e.activation(out=breT[:, di, :], in_=psb[0][:, 0:128], func=AF.Copy), [psb[0]], [breT])
        V("dve", lambda e, di=di: e.tensor_scalar(out=tb2[:, :], in0=bre[:, di, :], scalar1=ni[:, di:di + 1], scalar2=None,
                                                  op0=ALU.mult), [bre, ni], [tb2])
        V("dve", lambda e, di=di: e.scalar_tensor_tensor(out=tb2[:, :], in0=bim[:, di, :], scalar=nr[:, di:di + 1],
                                                         in1=tb2[:, :], op0=ALU.mult, op1=ALU.add), [bim, nr, tb2], [tb2])
        V("pe", lambda e: e.transpose(out=psb[1][:, 0:128], in_=tb2[:, :], identity=idt[:, :]), [tb2, idt], [psb[1]])
        V("act", lambda e, di=di: e.activation(out=bimT[:, di, :], in_=psb[1][:, 0:128], func=AF.Copy), [psb[1]], [bimT])

    YA = k.sb([128, 2, NCH * Q], name="YA")
    YAv = [[Buf(YA.ap[:, c, ch * Q:(ch + 1) * Q], f"YA{c}_{ch}") for ch in range(NCH)] for c in range(2)]
    hbuf = [k.sb([128, 8, Q], name=f"hb{i}") for i in range(2)]
    hvs = [hb.views(8) for hb in hbuf]
    U = [k.sb([128, Q], name=f"U{c}") for c in range(2)]
    gre_ = [k.sb([128, Q], name=f"gre{i}") for i in range(2)]; gim_ = [k.sb([128, Q], name=f"gim{i}") for i in range(2)]
    Gre_ = [k.sb([128, Q], name=f"Gre{i}") for i in range(2)]; Gim_ = [k.sb([128, Q], name=f"Gim{i}") for i in range(2)]
    hre = [k.sb([128, Q], name=f"hre{i}") for i in range(2)]; nhim = [k.sb([128, Q], name=f"nhim{i}") for i in range(2)]
    t1_ = [k.sb([128, Q], name=f"t1_{i}") for i in range(2)]; t2_ = [k.sb([128, Q], name=f"t2_{i}") for i in range(2)]
    car = k.sb([128, 12, 2], name="car"); ct_ = [k.sb([128, 2], name=f"ct{i}") for i in range(2)]
    V("dve", lambda e: e.memset(car[:, :, :], 0.0), [], [car])
    og = [k.sb([128, 2, Q], name=f"og{i}") for i in range(2)]
    hi = 0
    for d in range(2):
        order = list(range(NCH)) if d == 0 else [0] + list(range(NCH - 1, 0, -1))
        rv = (lambda ap: ap[:, ::-1]) if d == 1 else (lambda ap: ap)
        for ch in order:
            hb = hbuf[hi % 2]
            hviews = hvs[hi % 2]
            hi += 1
            load_h(k, hall, hviews, hb, ch)

            def ev_u(mc, ps):
                V("act", lambda e, mc=mc, ps=ps: e.activation(out=R(U[mc][:, :]), in_=ps[:, 0:Q], func=AF.Copy), [ps], [U[mc]])
            linear(k, w_u, range(2), 8, hviews, Q, ev_u)
            def tile_body(i, V):
                gre, gim, Gre, Gim = gre_[i % 2], gim_[i % 2], Gre_[i % 2], Gim_[i % 2]
                t1, t2, ct = t1_[i % 2], t2_[i % 2], ct_[i % 2]
                di = d * 6 + i
                c = 0 if i < 4 else 1
                pb_re, pb_im = psb[(i % 2) * 2], psb[(i % 2) * 2 + 1]
                V("pe", lambda e, di=di, c=c, p=pb_re: e.matmul(p[:, 0:Q], breT[:, di, :], R(U[c][:, :]), start=True, stop=True),
                  [breT, U[c]], [pb_re])
                V("pe", lambda e, di=di, c=c, p=pb_im: e.matmul(p[:, 0:Q], bimT[:, di, :], R(U[c][:, :]), start=True, stop=True),
                  [bimT, U[c]], [pb_im])
                cs, sn = cosT[:, di, :], sinT[:, di, :]
                V("dve", lambda e, p=pb_re, cs=cs, rv=rv: e.tensor_tensor(out=t1[:, :], in0=rv(p[:, 0:Q]), in1=cs, op=ALU.mult), [pb_re, cosT], [t1])
                V("dve", lambda e, p=pb_im, sn=sn, rv=rv: e.tensor_tensor(out=t2[:, :], in0=rv(p[:, 0:Q]), in1=sn, op=ALU.mult), [pb_im, sinT], [t2])
                V("dve", lambda e: e.tensor_tensor(out=gre[:, :], in0=t1[:, :], in1=t2[:, :], op=ALU.add), [t1, t2], [gre])
                V("dve", lambda e, p=pb_im, cs=cs, rv=rv: e.tensor_tensor(out=t1[:, :], in0=rv(p[:, 0:Q]), in1=cs, op=ALU.mult), [pb_im, cosT], [t1])
                V("dve", lambda e, p=pb_re, sn=sn, rv=rv: e.tensor_tensor(out=t2[:, :], in0=rv(p[:, 0:Q]), in1=sn, op=ALU.mult), [pb_re, sinT], [t2])
                V("dve", lambda e: e.tensor_tensor(out=gim[:, :], in0=t1[:, :], in1=t2[:, :], op=ALU.subtract), [t1, t2], [gim])
                V("dve", lambda e, di=di: e.tensor_tensor_scan(out=Gre[:, :], data0=rr[:, di:di + 1].to_broadcast([128, Q]), data1=gre[:, :],
                                                               initial=car[:, di, 0:1], op0=ALU.mult, op1=ALU.add), [rr, gre, car], [Gre])
                V("dve", lambda e, di=di: e.tensor_tensor_scan(out=Gim[:, :], data0=rr[:, di:di + 1].to_broadcast([128, Q]), data1=gim[:, :],
                                                               initial=car[:, di, 1:2], op0=ALU.mult, op1=ALU.add), [rr, gim, car], [Gim])
                hr, nh = hre[i % 2], nhim[i % 2]
                V("dve", lambda e, cs=cs: e.tensor_tensor(out=t1[:, :], in0=Gre[:, :], in1=cs, op=ALU.mult), [Gre, cosT], [t1])
                V("dve", lambda e, sn=sn: e.tensor_tensor(out=t2[:, :], in0=Gim[:, :], in1=sn, op=ALU.mult), [Gim, sinT], [t2])
                V("dve", lambda e, hr=hr: e.tensor_tensor(out=R(hr[:, :]), in0=t1[:, :], in1=t2[:, :], op=ALU.subtract), [t1, t2], [hr])
                V("dve", lambda e, sn=sn: e.tensor_tensor(out=t1[:, :], in0=Gre[:, :], in1=sn, op=ALU.mult), [Gre, sinT], [t1])
                V("dve", lambda e, cs=cs: e.tensor_tensor(out=t2[:, :], in0=Gim[:, :], in1=cs, op=ALU.mult), [Gim, cosT], [t2])
                V("dve", lambda e, nh=nh: e.scalar_tensor_tensor(out=R(nh[:, :]), in0=t1[:, :], scalar=-1.0, in1=t2[:, :],
                                                                 op0=ALU.mult, op1=ALU.subtract), [t1, t2], [nh])
                hl = hr[:, Q - 1:Q].bitcast(F32) if False else hr[:, Q - 1:Q]
                nl = nh[:, Q - 1:Q]
                V("dve", lambda e, di=di, nl=nl: e.tensor_scalar(out=ct[:, 0:1], in0=nl, scalar1=sinT[:, di, 1:2], scalar2=None,
                                                                 op0=ALU.mult), [nh, sinT], [ct])
                V("dve", lambda e, di=di, hl=hl: e.scalar_tensor_tensor(out=car[:, di, 0:1], in0=hl, scalar=cosT[:, di, 1:2],
                                                                        in1=ct[:, 0:1], op0=ALU.mult, op1=ALU.add), [hr, cosT, ct], [car])
                V("dve", lambda e, di=di, nl=nl: e.tensor_scalar(out=ct[:, 1:2], in0=nl, scalar1=cosT[:, di, 1:2], scalar2=None,
                                                                 op0=ALU.mult), [nh, cosT], [ct])
                V("dve", lambda e, di=di, hl=hl: e.scalar_tensor_tensor(out=car[:, di, 1:2], in0=hl, scalar=sinT[:, di, 1:2],
                                                                        in1=ct[:, 1:2], op0=ALU.mult, op1=ALU.subtract), [hr, sinT, ct], [car])
                first = (i == 0) or (i == 4)
                last = (i == 3) or (i == 5)
                V("pe", lambda e, di=di, c=c, hr=hr, first=first: e.matmul(psy[c][:, 0:Q], cre[:, di, :], R(hr[:, :]), start=first, stop=False),
                  [cre, hr], [psy[c]])
                V("pe", lambda e, di=di, c=c, nh=nh, last=last: e.matmul(psy[c][:, 0:Q], cim[:, di, :], R(nh[:, :]), start=False, stop=last),
                  [cim, nh], [psy[c]])
            for ia in (0, 2, 4):
                lists = []
                for i in (ia, ia + 1):
                    cur = []
                    tile_body(i, lambda eng, fn, r, w, cur=cur: cur.append((eng, fn, r, w)))
                    lists.append(cur)
                for j in range(max(len(lists[0]), len(lists[1]))):
                    for cur in lists:
                        if j < len(cur):
                            V(*cur[j])
            for c in range(2):
                ya = YAv[c][ch]
                if d == 0:
                    V("dve", lambda e, c=c, ya=ya: e.scalar_tensor_tensor(out=ya[:, :], in0=U[c][:, :], scalar=dsk[:, c:c + 1],
                                                                          in1=psy[c][:, 0:Q], op0=ALU.mult, op1=ALU.add), [U[c], dsk, psy[c]], [ya])
                else:
                    o = og[(hi) % 2]
                    V("dve", lambda e, c=c, ya=ya: e.tensor_tensor(out=ya[:, :], in0=ya[:, :], in1=psy[c][:, 0:Q][:, ::-1], op=ALU.add),
                      [ya, psy[c]], [ya])
                    V("act", lambda e, c=c, ya=ya, o=o: e.activation(out=o[:, c, :], in_=ya[:, :], func=AF.Gelu), [ya], [o])
            if d == 1:
                k.dma(ymix, ydst(ymix, 0, ch), o, o[:], q="act")
    k.phase_end()


def phase_SSD(k, P, hall, ymix):
    k.phase_begin("SSD")
    w_xbc, w_dt, conv_w, conv_b = P["w_xbc"], P["w_dt"], P["sconv_w"], P["sconv_b"]
    alog, dtb, dskip, masks, ident = P["alog"], P["dtb"], P["sdskip"], P["masks"], P["ident"]
    V = lambda eng, fn, r, w: k.op(eng, fn, reads=r, writes=w)

    k.wbuf = [k.sb([128, 8, 128], F32R, name=f"wb{i}") for i in range(2)]
    k.wstg = [k.sb([128, 8, 128], name=f"wstg{i}") for i in range(4)]
    k.si = 0
    k.wi = 0
    k.psl = [k.ps([128, 512], name=f"psu{i}") for i in range(2)]
    k.pi = 0
    pst = k.ps([128, 512], name="pst")
    ptr = [k.psview(pst, pst.ap[:, i * 128:(i + 1) * 128], f"ptr{i}") for i in range(3)]
    pdt = k.psview(pst, pst.ap[:, 384:388], "pdt"); pcol = k.psview(pst, pst.ap[:, 392:396], "pcol")
    prow = k.ps([128, 512], name="prow")
    pss = k.ps([128, 512], name="pss")
    psc = k.psview(pss, pss.ap[:, 0:128], "psc"); pstate = k.psview(pss, pss.ap[:, 256:512], "pstate")
    pys = [k.ps([128, 512], name=f"py{i}") for i in range(2)]

    def ld(name, src, shape, q="sp", dt=F32):
        t = k.sb(list(shape), dt, name=name)
        k.dma(t, t[:], src, src[:], q=q)
        return t
    cw = ld("cw", conv_w, [128, 4, 4]); cb = ld("cb", conv_b, [128, 4])
    al = ld("al", alog, [128, 2, 4]); db = ld("db", dtb, [128, 2, 4]); dsk = ld("dsk", dskip, [128, 2])
    mk32 = ld("mk32", masks, [128, 2, 128]); mkr = ld("mkr", masks, [128, 2, 128], q="pool", dt=F32R)
    idt = ld("idt", ident, [128, 128]); idr = ld("idr", ident, [128, 128], q="pool", dt=F32R)
    wdt = ld("wdt", w_dt, [128, 8, 4], q="pool", dt=F32R)
    na = k.sb([128, 2, 4], name="na")
    V("act", lambda e: e.activation(out=na[:, :, :], in_=al[:, :, :], func=AF.Exp), [al], [na])
    V("dve", lambda e: e.tensor_single_scalar(out=na[:, :, :], in_=na[:, :, :], scalar=-1.0, op=ALU.mult), [na], [na])
    ones32 = k.sb([128, 128], name="ones32"); ones = k.sb([128, 128], F32R, name="ones")
    V("dve", lambda e: e.memset(ones32[:, :], 1.0), [], [ones32])
    V("act", lambda e: e.activation(out=ones[:, :], in_=ones32[:, :], func=AF.Copy), [ones32], [ones])

    YACC = k.sb([128, 2, NCH * Q], name="YACC")
    YV = [[Buf(YACC.ap[:, c, j * 128:(j + 1) * 128], f"Y{c}_{j}") for j in range(2 * NCH)] for c in range(2)]
    hbt = [k.sb([128, 8, HW_], name=f"hb{i}") for i in range(2)]
    hvs = [t.views(8) for t in hbt]
    XCt = k.sb([128, 4, Q], name="XC"); XC = XCt.views(4)
    cv = k.sb([128, Q], name="cv")
    dt_ = k.sb([128, 4], name="dt"); adt = k.sb([128, 4], name="adt"); acol = k.sb([128, 4], name="acol")
    dte = k.sb([128, 4], name="dte"); cd = k.sb([128, 4], name="cd")
    arhs = k.sb([128, 512], name="arhs"); eac = k.sb([128, 512], name="eac")
    M1 = k.sb([128, 128], name="M1"); Lt = [k.sb([128, 128], name=f"Lt{i}") for i in range(2)]
    Mt = [k.sb([128, 128], name=f"Mt{i}") for i in range(4)]; CE = [k.sb([128, 128], name=f"CE{i}") for i in range(4)]
    XDT = [k.sb([128, 128], name=f"XDT{i}") for i in range(4)]; XDTE = k.sb([128, 256], name="XDTE")
    Btok = k.sb([128, 128], name="Btok")
    HS32 = [k.sb([128, 64], name=f"HS32_{i}") for i in range(4)]; HSr = [k.sb([128, 128], name=f"HSr{i}") for i in range(4)]
    z128 = k.sb([128, 128], name="z128")
    V("dve", lambda e: e.memset(z128[:, :], 0.0), [], [z128])
    for h in range(4):
        V("act", lambda e, h=h: e.activation(out=R(XDT[h][:, :]), in_=z128[:, :], func=AF.Copy), [z128], [XDT[h]])
    og = [k.sb([128, 2, Q], name=f"og{i}") for i in range(2)]
    zt = k.sb([128, 8, 2], name="zt")
    V("dve", lambda e: e.memset(zt[:, :, :], 0.0), [], [zt])
    hi = 0
    for d in range(2):
        for h in range(4):
            V("act", lambda e, h=h: e.activation(out=R(HSr[h][:, :]), in_=z128[:, :], func=AF.Copy), [z128], [HSr[h]])
            V("dve", lambda e, h=h: e.memset(HS32[h][:, :], 0.0), [], [HS32[h]])
        order = list(range(NCH)) if d == 0 else [0] + list(range(NCH - 1, 0, -1))
        for ch in order:
            hv = hvs[hi % 2]; hb = hbt[hi % 2]; o = og[hi % 2]; hi += 1
            load_h(k, hall, hv, hb, ch, off=2, zt=zt)

            def ev_c(mc, ps):
                V("dve", lambda e, mc=mc, ps=ps: e.tensor_scalar(out=cv[:, :], in0=ps[:, 0:Q], scalar1=cw[:, mc, 0:1], scalar2=cb[:, mc:mc + 1],
                                                                 op0=ALU.mult, op1=ALU.add), [ps, cw, cb], [cv])
                for kk in range(1, 4):
                    V("dve", lambda e, mc=mc, ps=ps, kk=kk: e.scalar_tensor_tensor(out=cv[:, :], in0=ps[:, kk:kk + Q], scalar=cw[:, mc, kk:kk + 1],
                                                                                   in1=cv[:, :], op0=ALU.mult, op1=ALU.add), [ps, cw, cv], [cv])
                V("act", lambda e, mc=mc: e.activation(out=R(XC[mc][:, :]), in_=cv[:, :], func=AF.Silu), [cv], [XC[mc]])
            linear(k, w_xbc, range(4), 8, hv, HW_, ev_c)
            for sub in ((0, 1) if d == 0 else (1, 0)):
                j = ch * 2 + sub
                s0 = sub * 128
                for kc in range(8):
                    V("pe", lambda e, kc=kc, hv=hv, s0=s0: e.matmul(pdt[:, :], R(hv[kc][:, 2 + s0:2 + s0 + 128]), wdt[:, kc, :],
                                                                    start=(kc == 0), stop=(kc == 7)), [hv[kc], wdt], [pdt])
                V("dve", lambda e, d=d: e.tensor_tensor(out=dt_[:, :], in0=pdt[:, :], in1=db[:, d, :], op=ALU.add), [pdt, db], [dt_])
                V("act", lambda e: e.activation(out=dt_[:, :], in_=dt_[:, :], func=AF.Exp), [dt_], [dt_])
                V("act", lambda e: e.activation(out=dt_[:, :], in_=dt_[:, :], func=AF.Ln, bias=1.0, scale=1.0), [dt_], [dt_])
                V("dve", lambda e, d=d: e.tensor_tensor(out=R(adt[:, :]), in0=dt_[:, :], in1=na[:, d, :], op=ALU.mult), [dt_, na], [adt])
                V("pe", lambda e, d=d: e.matmul(pcol[:, :], mkr[:, d, :], R(adt[:, :]), start=True, stop=True), [mkr, adt], [pcol])
                V("act", lambda e: e.activation(out=acol[:, :], in_=pcol[:, :], func=AF.Copy), [pcol], [acol])
                for h in range(4):
                    V("dve", lambda e, h=h, d=d: e.tensor_scalar(out=R(arhs[:, h * 128:(h + 1) * 128]), in0=mk32[:, d, :], scalar1=adt[:, h:h + 1], scalar2=None,
                                                                 op0=ALU.mult), [mk32, adt], [arhs])
                V("pe", lambda e: e.matmul(prow[:, :], ones[:, :], R(arhs[:, :]), start=True, stop=True), [ones, arhs], [prow])
                V("act", lambda e: e.activation(out=eac[:, :], in_=prow[:, :], func=AF.Exp), [prow], [eac])
                tcol = 127 if d == 0 else 0
                totv = prow[:, :].rearrange("p (h l) -> p h l", h=4)[:, :, tcol]
                V("dve", lambda e, totv=totv: e.tensor_tensor(out=dte[:, :], in0=totv, in1=acol[:, :], op=ALU.subtract), [prow, acol], [dte])
                V("act", lambda e: e.activation(out=dte[:, :], in_=dte[:, :], func=AF.Exp), [dte], [dte])
                V("act", lambda e, totv=totv: e.activation(out=cd[:, :], in_=totv, func=AF.Exp), [prow], [cd])
                V("pe", lambda e, s0=s0: e.matmul(psc[:, :], R(XC[2][:, s0:s0 + 128]), R(XC[3][:, s0:s0 + 128]), start=True, stop=True),
                  [XC[2], XC[3]], [psc])
                V("dve", lambda e, d=d: e.tensor_tensor(out=M1[:, :], in0=psc[:, :], in1=mk32[:, d, :], op=ALU.mult), [psc, mk32], [M1])
                for t_i, src in enumerate((XC[0], XC[1], XC[2])):
                    V("pe", lambda e, t_i=t_i, src=src, s0=s0: e.transpose(out=ptr[t_i][:, :], in_=src[:, s0:s0 + 128], identity=idt[:, :]),
                      [src, idt], [ptr[t_i]])
                V("act", lambda e: e.activation(out=R(Btok[:, :]), in_=ptr[2][:, :], func=AF.Copy), [ptr[2]], [Btok])
                for h in range(4):
                    c, hb_ = h // 2, (h % 2) * 64
                    V("dve", lambda e, h=h, c=c, hb_=hb_: e.tensor_scalar(out=R(XDT[h][:, hb_:hb_ + 64]), in0=ptr[c][:, hb_:hb_ + 64],
                                                                          scalar1=dt_[:, h:h + 1], scalar2=None, op0=ALU.mult), [ptr[c], dt_], [XDT[h]])
                    V("dve", lambda e, h=h, c=c, hb_=hb_: e.tensor_scalar(out=R(XDTE[:, h * 64:h * 64 + 64]), in0=ptr[c][:, hb_:hb_ + 64],
                                                                          scalar1=dt_[:, h:h + 1], scalar2=dte[:, h:h + 1], op0=ALU.mult, op1=ALU.mult),
                      [ptr[c], dt_, dte], [XDTE])
                    lt = Lt[h % 2]
                    V("dve", lambda e, h=h, lt=lt: e.tensor_scalar(out=lt[:, :], in0=prow[:, h * 128:(h + 1) * 128], scalar1=acol[:, h:h + 1], scalar2=0.0,
                                                                   op0=ALU.subtract, op1=ALU.min), [prow, acol], [lt])
                    V("act", lambda e, lt=lt: e.activation(out=lt[:, :], in_=lt[:, :], func=AF.Exp), [lt], [lt])
                    V("dve", lambda e, h=h, lt=lt: e.tensor_tensor(out=R(Mt[h][:, :]), in0=lt[:, :], in1=M1[:, :], op=ALU.mult), [lt, M1], [Mt[h]])
                    V("pool", lambda e, h=h, s0=s0: e.tensor_tensor(out=R(CE[h][:, :]), in0=XC[3][:, s0:s0 + 128], in1=eac[:, h * 128:(h + 1) * 128], op=ALU.mult),
                      [XC[3], eac], [CE[h]])
                for c in range(2):
                    for hh in range(2):
                        h = c * 2 + hh
                        V("pe", lambda e, c=c, h=h, hh=hh: e.matmul(pys[c][:, 0:128], R(XDT[h][:, :]), R(Mt[h][:, :]), start=(hh == 0), stop=False),
                          [XDT[h], Mt[h]], [pys[c]])
                        V("pe", lambda e, c=c, h=h, hh=hh: e.matmul(pys[c][:, 0:128], R(HSr[h][:, :]), R(CE[h][:, :]), start=False, stop=(hh == 1)),
                          [HSr[h], CE[h]], [pys[c]])
                    yv = YV[c][j]
                    if d == 0:
                        V("dve", lambda e, c=c, yv=yv, s0=s0: e.scalar_tensor_tensor(out=yv[:, :], in0=XC[c][:, s0:s0 + 128], scalar=dsk[:, c:c + 1],
                                                                                     in1=pys[c][:, 0:128], op0=ALU.mult, op1=ALU.add), [XC[c], dsk, pys[c]], [yv])
                    else:
                        V("dve", lambda e, c=c, yv=yv, s0=s0, o=o: e.tensor_tensor(out=o[:, c, s0:s0 + 128], in0=yv[:, :], in1=pys[c][:, 0:128], op=ALU.add),
                          [yv, pys[c]], [o])
                V("pe", lambda e: e.matmul(pstate[:, :], R(Btok[:, :]), R(XDTE[:, :]), start=True, stop=True), [Btok, XDTE], [pstate])
                for h in range(4):
                    hb_ = (h % 2) * 64
                    V("dve", lambda e, h=h: e.scalar_tensor_tensor(out=HS32[h][:, :], in0=HS32[h][:, :], scalar=cd[:, h:h + 1],
                                                                   in1=pstate[:, h * 64:(h + 1) * 64], op0=ALU.mult, op1=ALU.add), [HS32[h], cd, pstate], [HS32[h]])
                    V("act", lambda e, h=h, hb_=hb_: e.activation(out=R(HSr[h][:, hb_:hb_ + 64]), in_=HS32[h][:, :], func=AF.Copy), [HS32[h]], [HSr[h]])
            if d == 1:
                k.dma(ymix, ydst(ymix, 2, ch), o, o[:], q="act")
    k.phase_end()


OFF = [2] + [261 + (ch - 1) * Q for ch in range(1, NCH)]
XW = 261 + 32 * Q + 3


def phase_LRU(k, P, hall, ymix):
    k.phase_begin("LRU")
    w_x, w_gt, conv_w, conv_b, WG, BG, lam = P["w_x"], P["w_gt"], P["lconv_w"], P["lconv_b"], P["WG"], P["BG"], P["llam"]
    V = lambda eng, fn, r, w: k.op(eng, fn, reads=r, writes=w)

    k.wbuf = [k.sb([128, 8, 128], F32R, name=f"wb{i}") for i in range(2)]
    k.wstg = [k.sb([128, 8, 128], name=f"wstg{i}") for i in range(4)]
    k.si = 0
    k.wi = 0
    k.psl = [k.ps([128, 512], name=f"psu{i}") for i in range(4)]
    k.pi = 0
    psg = [k.ps([128, 512], name=f"psg{i}") for i in range(4)]

    def ld(name, src, shape, q="sp", dt=F32):
        t = k.sb(list(shape), dt, name=name)
        k.dma(t, t[:], src, src[:], q=q)
        return t
    cw = ld("cw", conv_w, [128, 2, 4]); cb = ld("cb", conv_b, [128, 2])
    wg = ld("wg", WG, [128, 8, 128], q="pool", dt=F32R); bg = ld("bg", BG, [128, 8]); lm = ld("lm", lam, [128, 4])
    ls8 = k.sb([128, 4], name="ls8"); ls16 = k.sb([128, 4], name="ls16")
    V("act", lambda e: e.activation(out=ls8[:, :], in_=lm[:, :], func=AF.Exp, scale=-1.0), [lm], [ls8])
    V("act", lambda e: e.activation(out=ls8[:, :], in_=ls8[:, :], func=AF.Ln, bias=1.0, scale=1.0), [ls8], [ls8])
    V("dve", lambda e: e.tensor_single_scalar(out=ls16[:, :], in_=ls8[:, :], scalar=-16.0, op=ALU.mult), [ls8], [ls16])
    V("dve", lambda e: e.tensor_single_scalar(out=ls8[:, :], in_=ls8[:, :], scalar=-8.0, op=ALU.mult), [ls8], [ls8])

    XA = k.sb([128, 2, XW + 128], name="XA")
    V("pool", lambda e: e.memset(XA[:, :, :], 0.0), [], [XA])
    HF = k.sb([128, 2, NCH * Q + 128], name="HF")
    HFv = [[Buf(HF.ap[:, c, ch * Q:(ch + 1) * Q], f"HF{c}_{ch}") for ch in range(NCH)] for c in range(2)]
    hbt = [k.sb([128, 8, Q], name=f"hb{i}") for i in range(2)]
    hvs = [t.views(8) for t in hbt]
    hi = 0
    for ch in range(NCH):
        hv = hvs[hi % 2]; hb = hbt[hi % 2]; hi += 1
        load_h(k, hall, hv, hb, ch)

        def ev_x(mc, ps, ch=ch):
            if ch == 0:
                V("act", lambda e, mc=mc, ps=ps: e.activation(out=XA[:, mc, 2:2 + Q], in_=ps[:, 0:Q], func=AF.Copy), [ps], [XA])
            else:
                base = 261 + 4 * (ch - 1)
                ov = XA.ap[:, mc, base:base + 8192].rearrange("p (j i) -> p i j", i=128)[:, 0:4, :]
                V("act", lambda e, ov=ov, ps=ps: e.activation(out=ov, in_=ps[:, 0:Q].rearrange("p (i j) -> p i j", j=64), func=AF.Copy),
                  [ps], [XA])
        linear(k, w_x, range(2), 8, hv, Q, ev_x)

    xc = [k.sb([128, Q], name=f"xc{i}") for i in range(2)]
    xcr = [k.sb([128, Q], name=f"xcr{i}") for i in range(2)]
    ra = [k.sb([128, Q], name=f"ra{i}") for i in range(2)]; ix = [k.sb([128, Q], name=f"ix{i}") for i in range(2)]
    At = [k.sb([128, Q], name=f"A{i}") for i in range(2)]; sq = [k.sb([128, Q], name=f"sq{i}") for i in range(2)]
    HB = [[k.sb([128, Q], name=f"HB{c}_{i}") for i in range(2)] for c in range(2)]
    gg = [k.sb([128, Q], name=f"gg{i}") for i in range(2)]
    og = [k.sb([128, 2, Q], name=f"og{i}") for i in range(2)]
    step = 0
    for d in range(2):
        order = list(range(NCH)) if d == 0 else [0] + list(range(NCH - 1, 0, -1))
        prev = None
        for oi, ch in enumerate(order):
            step += 1
            def tile_body(c, V):
                x_ = xc[c]
                o0 = OFF[ch]
                V("dve", lambda e, c=c, x_=x_, o0=o0: e.tensor_scalar(out=x_[:, :], in0=XA[:, c, o0 - 2:o0 - 2 + Q], scalar1=cw[:, c, 0:1],
                                                                     scalar2=cb[:, c:c + 1], op0=ALU.mult, op1=ALU.add), [XA, cw, cb], [x_])
                for kk in range(1, 4):
                    ov = R(xcr[c][:, :]) if kk == 3 else x_[:, :]
                    ob = xcr[c] if kk == 3 else x_
                    V("dve", lambda e, c=c, x_=x_, o0=o0, kk=kk, ov=ov: e.scalar_tensor_tensor(
                        out=ov, in0=XA[:, c, o0 - 2 + kk:o0 - 2 + kk + Q], scalar=cw[:, c, kk:kk + 1], in1=x_[:, :],
                        op0=ALU.mult, op1=ALU.add), [XA, cw, x_], [ob])
                x_ = xcr[c]
                pa, px = psg[c * 2], psg[c * 2 + 1]
                ia, ixx = (d * 2 + 0) * 2 + c, (d * 2 + 1) * 2 + c
                V("pe", lambda e, pa=pa, ia=ia, x_=x_: e.matmul(pa[:, 0:Q], wg[:, ia, :], R(x_[:, :]), start=True, stop=True), [wg, x_], [pa])
                V("pe", lambda e, px=px, ixx=ixx, x_=x_: e.matmul(px[:, 0:Q], wg[:, ixx, :], R(x_[:, :]), start=True, stop=True), [wg, x_], [px])
                V("act", lambda e, c=c, pa=pa, ia=ia: e.activation(out=ra[c][:, :], in_=pa[:, 0:Q], func=AF.Sigmoid, bias=bg[:, ia:ia + 1], scale=1.0),
                  [pa, bg], [ra[c]])
                V("act", lambda e, c=c, px=px, ixx=ixx: e.activation(out=ix[c][:, :], in_=px[:, 0:Q], func=AF.Sigmoid, bias=bg[:, ixx:ixx + 1], scale=1.0),
                  [px, bg], [ix[c]])
                li = d * 2 + c
                V("act", lambda e, c=c, li=li: e.activation(out=At[c][:, :], in_=ra[c][:, :], func=AF.Exp, scale=ls8[:, li:li + 1]), [ra[c], ls8], [At[c]])
                V("act", lambda e, c=c, li=li: e.activation(out=sq[c][:, :], in_=ra[c][:, :], func=AF.Exp, scale=ls16[:, li:li + 1]), [ra[c], ls16], [sq[c]])
                V("act", lambda e, c=c: e.activation(out=sq[c][:, :], in_=sq[c][:, :], func=AF.Sqrt, scale=-1.0, bias=1.0), [sq[c]], [sq[c]])
                V("dve", lambda e, c=c: e.tensor_tensor(out=sq[c][:, :], in0=sq[c][:, :], in1=ix[c][:, :], op=ALU.mult), [sq[c], ix[c]], [sq[c]])
                V("dve", lambda e, c=c, x_=x_: e.tensor_tensor(out=sq[c][:, :], in0=sq[c][:, :], in1=x_[:, :], op=ALU.mult), [sq[c], x_], [sq[c]])
                if d == 0:
                    dst = HFv[c][ch]
                    if prev is None:
                        ini, inib = 0.0, []
                    else:
                        ini, inib = HFv[c][prev][:, Q - 1:Q], [HFv[c][prev]]
                    V("dve", lambda e, c=c, dst=dst, ini=ini: e.tensor_tensor_scan(out=dst[:, :], data0=At[c][:, :], data1=sq[c][:, :], initial=ini,
                                                                                  op0=ALU.mult, op1=ALU.add), [At[c], sq[c]] + inib, [dst])
                else:
                    dst = HB[c][oi % 2]
                    if prev is None:
                        ini, inib = 0.0, []
                    else:
                        pb = HB[c][(oi - 1) % 2]
                        ini, inib = pb[:, 0:1], [pb]
                    V("dve", lambda e, c=c, dst=dst, ini=ini: e.tensor_tensor_scan(out=dst[:, ::-1], data0=At[c][:, ::-1], data1=sq[c][:, ::-1],
                                                                                  initial=ini, op0=ALU.mult, op1=ALU.add), [At[c], sq[c]] + inib, [dst])
            lists = []
            for c in range(2):
                cur = []
                tile_body(c, lambda eng, fn, r, w, cur=cur: cur.append((eng, fn, r, w)))
                lists.append(cur)
            for j in range(max(len(lists[0]), len(lists[1]))):
                for cur in lists:
                    if j < len(cur):
                        V(*cur[j])
            if d == 1:
                for c in range(2):
                    dst = HB[c][oi % 2]
                    V("pool", lambda e, c=c, dst=dst, ch=ch: e.tensor_tensor(out=HFv[c][ch][:, :], in0=HFv[c][ch][:, :], in1=dst[:, :], op=ALU.add),
                      [HFv[c][ch], dst], [HFv[c][ch]])
            prev = ch
    for ch in range(NCH):
        hv = hvs[hi % 2]; hb = hbt[hi % 2]; o = og[hi % 2]; hi += 1
        load_h(k, hall, hv, hb, ch)

        def ev_g(mc, ps, ch=ch, o=o):
            V("act", lambda e, mc=mc, ps=ps: e.activation(out=gg[mc][:, :], in_=ps[:, 0:Q], func=AF.Gelu), [ps], [gg[mc]])
            if ch == 0:
                V("dve", lambda e, mc=mc, o=o: e.tensor_tensor(out=o[:, mc, :], in0=HF[:, mc, 0:Q], in1=gg[mc][:, :], op=ALU.mult),
                  HFv[mc] + [gg[mc]], [o])
            else:
                base = 256 + 4 * (ch - 1)
                hvw = HF.ap[:, mc, base:base + 8192].rearrange("p (j i) -> p i j", i=128)[:, 0:4, :]
                V("dve", lambda e, mc=mc, o=o, hvw=hvw: e.tensor_tensor(out=o[:, mc, :].rearrange("p (i j) -> p i j", j=64), in0=hvw,
                                                                        in1=gg[mc][:, :].rearrange("p (i j) -> p i j", j=64), op=ALU.mult),
                  HFv[mc] + [gg[mc]], [o])
        linear(k, w_gt, range(2), 8, hv, Q, ev_g)
        k.dma(ymix, ydst(ymix, 1, ch), o, o[:], q="act")
    k.phase_end()


def wrel(w, pad_k=None):
    K, M = w.shape
    Kp = -(-K // 128) * 128; Mp = -(-M // 128) * 128
    if (Kp, Mp) != (K, M):
        wp = np.zeros((Kp, Mp), w.dtype); wp[:K, :M] = w; w = wp
    return np.ascontiguousarray(w.reshape(Kp // 128, 128, Mp // 128, 128).transpose(2, 1, 0, 3))

def vrel(v):
    return np.ascontiguousarray(v.reshape(-1, 128).T)

def fm_blocks(lat, ctx, NB=4):
    T, F = lat.shape
    tb = T // NB
    out = []
    for b in range(NB):
        t = lat[b * tb:(b + 1) * tb]
        if ctx is not None:
            cb = ctx.shape[0] // NB
            t = np.concatenate([t, ctx[b * cb:(b + 1) * cb]], 0)
        out.append(t.T.reshape(F // 128, 128, -1).transpose(1, 0, 2))
    return np.ascontiguousarray(np.stack(out))

def un_blocks(a, has_ctx, NB=4):
    NB, P, C, W = a.shape
    t = a.transpose(0, 3, 2, 1).reshape(NB, W, C * P)
    lat = t[:, :512].reshape(NB * 512, C * P)
    ctx = t[:, 512:].reshape(-1, C * P) if has_ctx else None
    return lat, ctx

def seq_chunks(hfull, Q=256):
    T, F = hfull.shape
    return np.ascontiguousarray(hfull.reshape(T // Q, Q, F // 128, 128).transpose(0, 3, 2, 1))

def s5_params(inp, l, q):
    lamre = np.zeros((128, 12), np.float32); lamim = np.zeros((128, 12), np.float32); logdt = np.zeros((128, 12), np.float32)
    Bre = np.zeros((128, 12, 128), np.float32); Bim = np.zeros_like(Bre); CreT = np.zeros_like(Bre); CimT = np.zeros_like(Bre)
    for d in range(2):
        for i in range(6):
            di = d * 6 + i
            for gg in range(2):
                g = 12 * q + 2 * i + gg
                ps = slice(gg * 64, gg * 64 + 64)
                lamre[ps, di] = inp["s5_lam_re"][l, d, g]; lamim[ps, di] = inp["s5_lam_im"][l, d, g]
                logdt[ps, di] = inp["s5_log_dt"][l, d, g]
                c0 = 32 * (i % 4) + gg * 16
                Bre[ps, di, c0:c0 + 16] = inp["s5_b_re"][l, d, g]; Bim[ps, di, c0:c0 + 16] = inp["s5_b_im"][l, d, g]
                CreT[ps, di, c0:c0 + 16] = inp["s5_c_re"][l, d, g].T; CimT[ps, di, c0:c0 + 16] = inp["s5_c_im"][l, d, g].T
    dsk = np.zeros((128, 2), np.float32)
    dv = inp["s5_d"][l, 192 * q:192 * q + 192]
    dsk[:, 0] = dv[:128]; dsk[:64, 1] = dv[128:]
    return dict(lamre=lamre, lamim=lamim, logdt=logdt, Bre=Bre, Bim=Bim, CreT=CreT, CimT=CimT, dskip=dsk,
                w_u=wrel(inp["w_in"][l][:, 192 * q:192 * q + 192]))

def to_cm(lat):
    T, F = lat.shape
    return lat.reshape(128, 64, F).transpose(1, 0, 2).reshape(T, F)

def from_cm(o):
    T, F = o.shape
    return o.reshape(64, 128, F).transpose(1, 0, 2).reshape(T, F)

def lru_params(inp, l, q):
    WG = np.zeros((128, 8, 128), np.float32); BG = np.zeros((128, 8), np.float32); lam = np.zeros((128, 4), np.float32)
    for d in range(2):
        for kind, (wn, bn) in enumerate((("lru_w_a", "lru_b_a"), ("lru_w_x", "lru_b_x"))):
            for c in range(2):
                idx = (d * 2 + kind) * 2 + c
                for bb in range(2):
                    blk = 4 * q + 2 * c + bb
                    WG[bb * 64:(bb + 1) * 64, idx, bb * 64:(bb + 1) * 64] = inp[wn][l, d, blk]
                BG[:, idx] = inp[bn][l, d, 256 * q + 128 * c:256 * q + 128 * c + 128]
        for c in range(2):
            lam[:, d * 2 + c] = inp["lru_lam"][l, d, 256 * q + 128 * c:256 * q + 128 * c + 128]
    ch = slice(256 * q, 256 * q + 256)
    cw = np.ascontiguousarray(inp["lru_conv_w"][l][:, ch].T.reshape(2, 128, 4).transpose(1, 0, 2))
    cb = np.ascontiguousarray(inp["lru_conv_b"][l][ch].reshape(2, 128).T)
    return dict(WG=WG, BG=BG, llam=lam, lconv_w=cw, lconv_b=cb,
                w_x=wrel(inp["w_in"][l][:, 768 + 256 * q:768 + 256 * q + 256]),
                w_gt=wrel(inp["w_in"][l][:, 1792 + 256 * q:1792 + 256 * q + 256]))

def halo_chunks(hctx, hlat, Q=256):
    F = hctx.shape[1]
    out = np.zeros((33, Q + 4, F), np.float32)
    out[0, 2:2 + Q] = hctx
    lp = np.zeros((hlat.shape[0] + 4, F), np.float32); lp[2:-2] = hlat
    for ch in range(32):
        out[ch + 1] = lp[ch * Q: ch * Q + Q + 4]
    return np.ascontiguousarray(out.reshape(33, Q + 4, F // 128, 128).transpose(0, 3, 2, 1))

def ssd_params(inp, l, q):
    W = inp["w_in"][l]
    xs = 3840 + 256 * q; bs = 3840 + 1024 + 128 * q; cs = 3840 + 1536 + 128 * q
    wx = np.concatenate([W[:, xs:xs + 256], W[:, bs:bs + 128], W[:, cs:cs + 128]], 1)
    chans = np.concatenate([np.arange(256 * q, 256 * q + 256), np.arange(1024 + 128 * q, 1024 + 128 * q + 128),
                            np.arange(1536 + 128 * q, 1536 + 128 * q + 128)])
    cw = np.ascontiguousarray(inp["ssd_conv_w"][l][:, chans].T.reshape(4, 128, 4).transpose(1, 0, 2))
    cb = np.ascontiguousarray(inp["ssd_conv_b"][l][chans].reshape(4, 128).T)
    wdt = np.ascontiguousarray(W[:, 5888 + 4 * q:5888 + 4 * q + 4].reshape(8, 128, 4).transpose(1, 0, 2))
    alog = np.ascontiguousarray(np.broadcast_to(inp["ssd_a_log"][l][:, 4 * q:4 * q + 4], (128, 2, 4)))
    dtb = np.ascontiguousarray(np.broadcast_to(inp["ssd_dt_bias"][l][:, 4 * q:4 * q + 4], (128, 2, 4)))
    dsk = np.ascontiguousarray(np.repeat(inp["ssd_d"][l][4 * q:4 * q + 4], 64).reshape(2, 128).T)
    masks = np.ascontiguousarray(np.stack([np.triu(np.ones((128, 128), np.float32)), np.tril(np.ones((128, 128), np.float32))], 1))
    return dict(w_xbc=wrel(wx), w_dt=wdt, sconv_w=cw, sconv_b=cb, alog=alog, dtb=dtb, sdskip=dsk)


SHAPES_MIX = dict(w_u=[2, 128, 8, 128], lamre=[128, 12], lamim=[128, 12], logdt=[128, 12], Bre=[128, 12, 128], Bim=[128, 12, 128],
                  CreT=[128, 12, 128], CimT=[128, 12, 128], dskip=[128, 2],
                  w_x=[2, 128, 8, 128], w_gt=[2, 128, 8, 128], lconv_w=[128, 2, 4], lconv_b=[128, 2], WG=[128, 8, 128], BG=[128, 8],
                  llam=[128, 4],
                  w_xbc=[4, 128, 8, 128], w_dt=[128, 8, 4], sconv_w=[128, 4, 4], sconv_b=[128, 4], alog=[128, 2, 4], dtb=[128, 2, 4],
                  sdskip=[128, 2])
SHAPES_C = dict(w_z=[8, 128, 8, 128], w_g=[24, 128, 8, 128], w_glu=[6, 128, 6, 128], w_bra=[8, 128, 6, 128], w_brb=[8, 128, 8, 128],
                w_brc=[8, 128, 8, 128], w_out=[8, 128, 8, 128], ssd_nw=[128, 8], w_mod=[48, 128, 8, 128], b_mod=[128, 48],
                nw=[128, 2, 8])
SHAPES_C0 = dict(w1=[22, 128, 8, 128], w3=[22, 128, 8, 128], w2=[8, 128, 22, 128], w_modn=[16, 128, 8, 128], b_modn=[128, 16], nwn=[128, 8])
SHAPES_C1 = dict(w1=[224, 128, 8, 128], w3=[224, 128, 8, 128], w2=[64, 128, 28, 128], wr=[128, 8, 8], br=[128, 8], fnw=[128, 8])
SHAPES_A0 = dict(w_modn=[16, 128, 8, 128], b_modn=[128, 16], nwn=[128, 8])
SHAPES_CONST = dict(cond=[128, 8, 2], iota=[128, 256], ident=[128, 128], masks=[128, 2, 128], sel=[8, 8, 128])


def build_fused():
    nc = bass.Bass("TRN2", target_bir_lowering=False)
    k = K(nc)
    qv = nc.partition_id() % 4

    def ins(shapes, suffix):
        return {n: k.dram(n + suffix, s, kind="ExternalInput") for n, s in shapes.items()}
    xT = k.dram("xT", [4, 128, 8, 576], kind="ExternalInput")
    CONST = ins(SHAPES_CONST, "")
    PA = dict(CONST); PA.update(ins(SHAPES_A0, "_a"))
    PM = []; PC = []
    for l in range(2):
        pm = dict(CONST); pm.update(ins(SHAPES_MIX, f"_m{l}")); PM.append(pm)
        pc = dict(CONST); pc.update(ins(SHAPES_C, f"_c{l}")); pc.update(ins(SHAPES_C0 if l == 0 else SHAPES_C1, f"_c{l}")); PC.append(pc)
    oT = k.dram("oT", [4, 128, 8, 512], kind="ExternalOutput")
    hblk = [k.dram(f"hblk{i}", [4, 128, 8, 576]) for i in range(2)]
    xblk = k.dram("xblk", [4, 128, 8, 576])
    hall = k.dram("hall", [4, 4, 4, 32, 8, 576])
    ymix = k.dram("ymix", [3, 17, 128, 2, 512])
    yall = k.dram("yall", [3, 17, 4, 128, 2, 512])
    ystage = [k.dram(f"ystage{i}", [3, 4, 128, 2, 512]) for i in range(2)]
    for b_ in ystage:
        b_.track = True

    def gather(pieces):
        k.phase_begin("G")
        sem = k._sem()
        for (src_ap, dst_ap) in pieces:
            sb_ = Buf(src_ap, "gsrc"); sb_.dram = True
            db_ = Buf(dst_ap, "gdst"); db_.dram = True
            k.collective("AllGather", sb_, db_, GROUPS, sem=sem)
        k.phase_end()

    import os
    stop = int(os.environ.get("KF_STOP", "99"))
    dbg = os.environ.get("KF_DEBUG")

    def dcopy(name, src):
        d = k.dram(name, list(src.ap.shape), kind="ExternalOutput")
        k.phase_begin("D")
        k.dma(d, d.ap.opt() if False else d.ap, src, src.ap)
        k.phase_end()
    cc_h = k.cc_sem("cc_h"); cc_y = k.cc_sem("cc_y")
    phase_C(k, PA, False, None, True, "h_next", xT, None, None, qv, hblk[0], None, None, hgather=(hall, cc_h))
    for l in range(2):
        if l == 1:
            k.renew_engine_sems()
        k.cc_wait(cc_h)
        for m_, ph in enumerate((phase_S5, phase_LRU, phase_SSD)):
            ph(k, PM[l], hall, ymix)
            k.gather_async([(ymix.ap[m_, t_], yall.ap[m_, t_]) for t_ in range(17)], cc_y)
        k.cc_wait(cc_y)
        if l == 0:
            phase_C(k, PC[0], True, "dense", True, "h_next", xT, hblk[0], yall, qv, hblk[1], xblk, None, ystage, hgather=(hall, cc_h))
        else:
            k.renew_engine_sems()
            phase_C(k, PC[1], True, "moe", False, "final", xblk, hblk[1], yall, qv, None, None, oT, ystage)
    return nc


_NC_CACHE = {}


def kernel(**inp):
    inp = {k_: np.asarray(v, dtype=np.float32) for k_, v in inp.items()}
    x, c, ctx, c_ctx = inp["x"], inp["c"], inp["ctx"], inp["c_ctx"]
    cores = [(co // 4, co % 4) for co in range(8)]
    if "nc" not in _NC_CACHE:
        _NC_CACHE["nc"] = build_fused()
    nc = _NC_CACHE["nc"]
    sel = np.zeros((8, 8, 128), np.float32)
    for e_ in range(8):
        sel[e_, e_, :] = 1.0
    const = {"iota": np.ascontiguousarray(np.broadcast_to(np.arange(256, dtype=np.float32), (128, 256))),
             "ident": np.eye(128, dtype=np.float32), "sel": sel,
             "masks": np.ascontiguousarray(np.stack([np.triu(np.ones((128, 128), np.float32)), np.tril(np.ones((128, 128), np.float32))], 1))}
    shared = {}
    shared.update({"w_modn_a": wrel(inp["w_mod"][0][:, :2048]), "b_modn_a": vrel(inp["b_mod"][0][:2048]), "nwn_a": vrel(inp["norm_w"][0, 0])})
    for l in range(2):
        W_in = inp["w_in"][l]
        cw = {"w_z": wrel(W_in[:, 2816:3840]), "w_g": wrel(W_in[:, 5904:8976]), "w_glu": wrel(inp["s5_w_glu"][l]),
              "w_bra": wrel(inp["w_br_a"][l]), "w_brb": wrel(inp["w_br_b"][l]), "w_brc": wrel(inp["w_br_c"][l]),
              "w_out": wrel(inp["w_out"][l]), "ssd_nw": vrel(inp["ssd_norm_w"][l]), "w_mod": wrel(inp["w_mod"][l]),
              "b_mod": vrel(inp["b_mod"][l]), "nw": np.ascontiguousarray(inp["norm_w"][l].reshape(2, 8, 128).transpose(2, 0, 1))}
        if l == 0:
            cw.update({"w1": wrel(inp["ffn_w1"][0]), "w3": wrel(inp["ffn_w3"][0]), "w2": wrel(inp["ffn_w2"][0]),
                       "w_modn": wrel(inp["w_mod"][1][:, :2048]), "b_modn": vrel(inp["b_mod"][1][:2048]), "nwn": vrel(inp["norm_w"][1, 0])})
        else:
            cw.update({"w1": np.concatenate([wrel(inp["moe_w1"][0, e_]) for e_ in range(8)], 0),
                       "w3": np.concatenate([wrel(inp["moe_w3"][0, e_]) for e_ in range(8)], 0),
                       "w2": np.concatenate([wrel(inp["moe_w2"][0, e_]) for e_ in range(8)], 0),
                       "wr": np.ascontiguousarray(inp["moe_w_router"][0].reshape(8, 128, 8).transpose(1, 0, 2)),
                       "br": np.ascontiguousarray(np.broadcast_to(inp["moe_b_router"][0], (128, 8))), "fnw": vrel(inp["final_norm_w"])})
        shared.update({n + f"_c{l}": v for n, v in cw.items()})
    maps = []
    for (b, q) in cores:
        m = dict(const); m.update(shared)
        m["xT"] = fm_blocks(x[b, q * 2048:(q + 1) * 2048], ctx[b])
        m["cond"] = np.ascontiguousarray(np.stack([vrel(c[b]), vrel(c_ctx)], -1))
        for l in range(2):
            pm = {}
            pm.update(s5_params(inp, l, q)); pm.update(lru_params(inp, l, q)); pm.update(ssd_params(inp, l, q))
            m.update({n + f"_m{l}": v for n, v in pm.items()})
        maps.append(m)
    res = run_bass_kernel_spmd(nc, maps, core_ids=list(range(8))).results
    _NC_CACHE["res"] = res
    out = np.zeros((2, 8192, 1024), np.float32)
    for co, (b, q) in enumerate(cores):
        la, _ = un_blocks(res[co]["oT"], False)
        out[b, q * 2048:(q + 1) * 2048] = la
    return out
```
